# Optimizing a Trainium2 kernel written in Bass

```python
import math
import jax, jax.numpy as jnp
from jax import lax
import numpy as np

D_MODEL = 2048
BATCH = 2
SEQ = 8192
DEPTH = 4

GLA_HEADS = 4
GLA_VDIM = D_MODEL // 2
GLA_KDIM = GLA_VDIM // 2
HEAD_K = GLA_KDIM // GLA_HEADS
HEAD_V = GLA_VDIM // GLA_HEADS
DECAY_RANK = 16
GATE_NORMALIZER = 16.0
CHUNK = 64
POOL_DIM = D_MODEL // 2
POOL_WINDOWS = (2, 4, 8, 16)
POOL_GROUPS = 4
POOL_GROUP_DIM = POOL_DIM // POOL_GROUPS
N_GROUPS = 8
EXPERTS_PER_GROUP = 8
N_EXPERTS = N_GROUPS * EXPERTS_PER_GROUP
TOP_K_FINE = 2
D_FF_EXPERT = D_MODEL // 8
MOE_BLOCK = 128
N_MOD = 6
EPS = 1e-6
IN_SPLITS = (GLA_KDIM, GLA_KDIM, GLA_VDIM, GLA_VDIM, 2 * DECAY_RANK, POOL_DIM, D_MODEL, D_MODEL)
IN_COLS = sum(IN_SPLITS)

kernel_name = 'hybrid_gla_pool_hmoe_encoder'


def rms_norm(x, g):
    x32 = x.astype(jnp.float32)
    y = x32 * lax.rsqrt(jnp.mean(x32 * x32, axis=-1, keepdims=True) + EPS)
    return y.astype(x.dtype) * g


def split_cols(t):
    out = []
    start = 0
    for w in IN_SPLITS:
        out.append(t[..., start:start + w])
        start += w
    return out


def to_heads(t, n_heads):
    b_, s_, w = t.shape
    return t.reshape(b_, s_, n_heads, w // n_heads).transpose(0, 2, 1, 3)


def gla_chunk_scan(q, k, v, log_a, strict):
    b_, h_, s_, dk = q.shape
    dv = v.shape[-1]
    n = s_ // CHUNK
    q = q.reshape(b_, h_, n, CHUNK, dk)
    k = k.reshape(b_, h_, n, CHUNK, dk)
    v = v.reshape(b_, h_, n, CHUNK, dv)
    cum = jnp.cumsum(log_a.reshape(b_, h_, n, CHUNK, dk), axis=3)
    last = cum[:, :, :, -1:, :]
    q_dec = q * jnp.exp(cum)
    k_inv = k * jnp.exp(-cum)
    k_end = k * jnp.exp(last - cum)
    mask = np.tril(np.ones((CHUNK, CHUNK), dtype=bool), k=-1 if strict else 0)
    att = jnp.einsum('bhncd,bhnsd->bhncs', q_dec, k_inv)
    att = jnp.where(mask, att, 0.0)
    o_intra = jnp.einsum('bhncs,bhnse->bhnce', att, v)
    chunk_decay = jnp.exp(last[:, :, :, 0, :])

    def step(state, inp):
        qd, ke, vv, dec = inp
        o = jnp.einsum('bhcd,bhde->bhce', qd, state)
        state = dec[..., None] * state + jnp.einsum('bhcd,bhce->bhde', ke, vv)
        return state, o

    xs = (jnp.moveaxis(q_dec, 2, 0), jnp.moveaxis(k_end, 2, 0),
          jnp.moveaxis(v, 2, 0), jnp.moveaxis(chunk_decay, 2, 0))
    state0 = jnp.zeros((b_, h_, dk, dv), jnp.float32)
    _, o_inter = lax.scan(step, state0, xs)
    o = o_intra + jnp.moveaxis(o_inter, 0, 2)
    return o.reshape(b_, h_, s_, dv)


def gla_mixer(q, k, v, r, lr, wf, bf, wb, bb, g_norm):
    dt = q.dtype
    f = lambda t: t.astype(jnp.float32)
    qh = to_heads(f(q), GLA_HEADS) * HEAD_K ** -0.5
    kh = to_heads(f(k), GLA_HEADS)
    vh = to_heads(f(v), GLA_HEADS)
    la_f = to_heads(jax.nn.log_sigmoid(f(lr[..., :DECAY_RANK]) @ f(wf) + f(bf)) / GATE_NORMALIZER, GLA_HEADS)
    la_b = to_heads(jax.nn.log_sigmoid(f(lr[..., DECAY_RANK:]) @ f(wb) + f(bb)) / GATE_NORMALIZER, GLA_HEADS)
    o_f = gla_chunk_scan(qh, kh, vh, la_f, strict=False)
    rev = lambda t: t[:, :, ::-1]
    o_b = rev(gla_chunk_scan(rev(qh), rev(kh), rev(vh), rev(la_b), strict=True))
    o = o_f + o_b
    o = o * lax.rsqrt(jnp.mean(o * o, axis=-1, keepdims=True) + EPS) * f(g_norm)
    b_, h_, s_, dv = o.shape
    o = o.transpose(0, 2, 1, 3).reshape(b_, s_, h_ * dv)
    return (o * jax.nn.silu(f(r))).astype(dt)


def multiscale_pool(u):
    b_, s_, g_, cg = u.shape
    cs = jnp.concatenate([jnp.zeros((b_, 1, g_, cg), u.dtype), jnp.cumsum(u, axis=1)], axis=1)
    t = jnp.arange(s_)
    outs = []
    for gi, w in enumerate(POOL_WINDOWS):
        lo = jnp.clip(t - w // 2, 0, s_)
        hi = jnp.clip(t + w // 2, 0, s_)
        cnt = (hi - lo).astype(u.dtype)
        csg = cs[:, :, gi]
        mean = (csg[:, hi] - csg[:, lo]) / cnt[None, :, None]
        outs.append(mean - u[:, :, gi])
    return jnp.stack(outs, axis=2)


def pool_mixer(u, w, b, scale):
    b_, s_, _ = u.shape
    u32 = u.astype(jnp.float32).reshape(b_, s_, POOL_GROUPS, POOL_GROUP_DIM)
    p = multiscale_pool(u32)
    y = jnp.einsum('bsgc,gcd->bsgd', p, w.astype(jnp.float32)).reshape(b_, s_, POOL_DIM)
    return ((y + b.astype(jnp.float32)) * scale.astype(jnp.float32)).astype(u.dtype)


def hier_moe(h, wc, bc, wf, bf, w1, w3, w2):
    b_, s_, d = h.shape
    t_ = b_ * s_
    hf = h.reshape(t_, d)
    coarse = (hf @ wc + bc).astype(jnp.float32)
    probs = jax.nn.softmax(coarse, axis=-1)
    g_star = jnp.argmax(coarse, axis=-1).astype(jnp.int32)
    p_g = jnp.take_along_axis(probs, g_star[:, None], axis=1)[:, 0]
    fine = (hf @ wf + bf).astype(jnp.float32).reshape(t_, N_GROUPS, EXPERTS_PER_GROUP)
    fine_sel = jnp.take_along_axis(fine, g_star[:, None, None], axis=1)[:, 0]
    top_v, top_i = lax.top_k(fine_sel, TOP_K_FINE)
    wts = jax.nn.softmax(top_v, axis=-1) * p_g[:, None]
    experts = g_star[:, None] * EXPERTS_PER_GROUP + top_i.astype(jnp.int32)
    n_assign = t_ * TOP_K_FINE
    n_blocks = -(-n_assign // MOE_BLOCK) + N_EXPERTS
    e_flat = experts.reshape(-1)
    w_flat = wts.reshape(-1)
    tok_flat = jnp.repeat(jnp.arange(t_, dtype=jnp.int32), TOP_K_FINE)
    counts = jnp.bincount(e_flat, length=N_EXPERTS)
    start = jnp.cumsum(counts) - counts
    padded = (counts + MOE_BLOCK - 1) // MOE_BLOCK * MOE_BLOCK
    pend = jnp.cumsum(padded)
    pstart = pend - padded
    order = jnp.argsort(e_flat, stable=True)
    e_sorted = e_flat[order]
    dest = pstart[e_sorted] + jnp.arange(n_assign) - start[e_sorted]
    n_slots = n_blocks * MOE_BLOCK
    slot_tok = jnp.full((n_slots,), t_, jnp.int32).at[dest].set(tok_flat[order])
    slot_w = jnp.zeros((n_slots,), h.dtype).at[dest].set(w_flat[order].astype(h.dtype))
    block_expert = jnp.minimum(
        jnp.searchsorted(pend // MOE_BLOCK, jnp.arange(n_blocks), side='right'), N_EXPERTS - 1)
    h_pad = jnp.concatenate([hf, jnp.zeros((1, d), h.dtype)], axis=0)
    xs = h_pad[slot_tok].reshape(n_blocks, MOE_BLOCK, d)

    def expert_block(args):
        e, xb = args
        return (jax.nn.silu(xb @ w1[e]) * (xb @ w3[e])) @ w2[e]

    ys = lax.map(expert_block, (block_expert, xs))
    y = jnp.zeros((t_ + 1, d), h.dtype).at[slot_tok].add(ys.reshape(-1, d) * slot_w[:, None])
    return y[:t_].reshape(b_, s_, d)


def setup_inputs(seed: int = 0) -> dict:
    key = jax.random.key(seed)
    ks = jax.random.split(key, 28)
    L, D = DEPTH, D_MODEL

    def nrm(k, shape, scale):
        return jax.random.normal(k, shape, jnp.float32) * scale

    return {
        'x': nrm(ks[0], (BATCH, SEQ, D), 1.0),
        'c': nrm(ks[1], (BATCH, D), 1.0),
        'ada_w': nrm(ks[2], (L, D, N_MOD * D), 0.5 * D ** -0.5),
        'ada_b': nrm(ks[3], (L, N_MOD * D), 0.02),
        'mix_norm_g': 1.0 + nrm(ks[4], (L, D), 0.02),
        'in_w': nrm(ks[5], (L, D, IN_COLS), D ** -0.5),
        'decay_fw_w': nrm(ks[6], (L, DECAY_RANK, GLA_KDIM), DECAY_RANK ** -0.5),
        'decay_fw_b': nrm(ks[7], (L, GLA_KDIM), 0.02),
        'decay_bw_w': nrm(ks[8], (L, DECAY_RANK, GLA_KDIM), DECAY_RANK ** -0.5),
        'decay_bw_b': nrm(ks[9], (L, GLA_KDIM), 0.02),
        'gla_norm_g': 1.0 + nrm(ks[10], (L, HEAD_V), 0.02),
        'pool_w': nrm(ks[11], (L, POOL_GROUPS, POOL_GROUP_DIM, POOL_GROUP_DIM), POOL_GROUP_DIM ** -0.5),
        'pool_b': nrm(ks[12], (L, POOL_DIM), 0.02),
        'pool_scale': 1.0 + nrm(ks[13], (L, POOL_DIM), 0.1),
        'branch_a_w': nrm(ks[14], (L, GLA_VDIM, D), GLA_VDIM ** -0.5),
        'branch_b_w': nrm(ks[15], (L, POOL_DIM, D), POOL_DIM ** -0.5),
        'out_w': nrm(ks[16], (L, D, D), D ** -0.5),
        'ffn_norm_g': 1.0 + nrm(ks[17], (L, D), 0.02),
        'router_coarse_w': nrm(ks[18], (L, D, N_GROUPS), D ** -0.5),
        'router_coarse_b': nrm(ks[19], (L, N_GROUPS), 0.01),
        'router_fine_w': nrm(ks[20], (L, D, N_EXPERTS), D ** -0.5),
        'router_fine_b': nrm(ks[21], (L, N_EXPERTS), 0.01),
        'expert_w1': nrm(ks[22], (L, N_EXPERTS, D, D_FF_EXPERT), D ** -0.5),
        'expert_w3': nrm(ks[23], (L, N_EXPERTS, D, D_FF_EXPERT), D ** -0.5),
        'expert_w2': nrm(ks[24], (L, N_EXPERTS, D_FF_EXPERT, D), D_FF_EXPERT ** -0.5),
        'final_norm_g': 1.0 + nrm(ks[25], (D,), 0.02),
    }


def reference(x, c, ada_w, ada_b, mix_norm_g, in_w, decay_fw_w, decay_fw_b, decay_bw_w, decay_bw_b,
              gla_norm_g, pool_w, pool_b, pool_scale, branch_a_w, branch_b_w, out_w, ffn_norm_g,
              router_coarse_w, router_coarse_b, router_fine_w, router_fine_b,
              expert_w1, expert_w3, expert_w2, final_norm_g):
    for l in range(DEPTH):
        mod = (jax.nn.silu(c) @ ada_w[l] + ada_b[l]).reshape(c.shape[0], N_MOD, D_MODEL)[:, :, None, :]
        shift1, scale1, gate1, shift2, scale2, gate2 = [mod[:, i] for i in range(N_MOD)]
        h = rms_norm(x, mix_norm_g[l]) * (1.0 + scale1) + shift1
        q, k, v, r, lr, u, ga, gb = split_cols(h @ in_w[l])
        y_a = gla_mixer(q, k, v, r, lr, decay_fw_w[l], decay_fw_b[l], decay_bw_w[l], decay_bw_b[l],
                        gla_norm_g[l]) @ branch_a_w[l]
        y_b = pool_mixer(u, pool_w[l], pool_b[l], pool_scale[l]) @ branch_b_w[l]
        merged = jax.nn.sigmoid(ga) * y_a + jax.nn.sigmoid(gb) * y_b
        x = x + gate1 * (merged @ out_w[l])
        h = rms_norm(x, ffn_norm_g[l]) * (1.0 + scale2) + shift2
        x = x + gate2 * hier_moe(h, router_coarse_w[l], router_coarse_b[l], router_fine_w[l],
                                 router_fine_b[l], expert_w1[l], expert_w3[l], expert_w2[l])
    return rms_norm(x, final_norm_g)
```

```python
import numpy as np
from contextlib import ExitStack
import concourse.bass as bass
import concourse.mybir as mybir
from concourse.bass_utils import run_bass_kernel_spmd

F32 = mybir.dt.float32
BF16 = mybir.dt.bfloat16
I32 = mybir.dt.int32
ALU = mybir.AluOpType
AF = mybir.ActivationFunctionType

D = 2048
NKC = 16
KDIM = 512
VDIM = 1024
PDIM = 1024
NEXP = 64
FF = 256
EPS = 1e-6
import os as _os
BLK = int(_os.environ.get('K_BLK', '256'))
NSUB = BLK // 128
SKIP_OOB = False
PRECAST = True
BIGNEG = 1.0e30
POOL_FILTER = None
import os as _os
INORDER = tuple(_os.environ.get('K_INORDER', 'pe').split(','))
RUN_KW = {}


class Buf:
    __slots__ = ("name", "t", "w", "r")

    def __init__(self, name, t):
        self.name, self.t, self.w, self.r = name, t, None, []


class Rec:
    __slots__ = ("kind", "eng", "fn", "deps", "needed", "sem", "sigval", "prewait")

    def __init__(self, kind, eng, fn):
        self.kind, self.eng, self.fn = kind, eng, fn
        self.deps = []
        self.needed = False
        self.sem = None
        self.sigval = None
        self.prewait = None


class Sched:
    ENGS = ('pe', 'act', 'dve', 'pool', 'sp')
    NDS = 8

    def __init__(self, nc):
        self.nc = nc
        self.prog = {k: [] for k in self.ENGS}
        self.es = None
        self.nbuf = 0
        self.since_barrier = {k: [] for k in self.ENGS}

    def nds(self, k):
        import os
        return int(os.environ.get("POOL_NDS", "8")) if k == 'pool' else self.NDS

    def sb(self, name, shape, dt, es=None):
        self.nbuf += 1
        t = (es or self.es).enter_context(self.nc.sbuf_tensor(f"{name}_{self.nbuf}", shape, dt))
        return Buf(name, t)

    def ps(self, name, shape, dt):
        self.nbuf += 1
        t = self.es.enter_context(self.nc.psum_tensor(f"{name}_{self.nbuf}", shape, dt))
        return Buf(name, t)

    def dram(self, name, ap):
        return Buf(name, ap)

    def _emit(self, kind, eng, reads, writes, fn, extra_deps=(), force=False):
        calls = []

        class _P:
            def __getattr__(self_, name):
                def f(*a, **k):
                    calls.append((name, a, k))
                    return None
                return f
        fn(_P())
        assert len(calls) == 1
        rec = Rec(kind, eng, calls[0])
        if not isinstance(reads, (list, tuple)):
            reads = [reads]
        if not isinstance(writes, (list, tuple)):
            writes = [writes]
        deps = list(extra_deps)
        for b in reads:
            if b.w is not None:
                deps.append(b.w)
        for b in writes:
            if b.w is not None:
                deps.append(b.w)
            deps.extend(b.r)
        seen = set()
        for d in deps:
            if id(d) in seen or d is rec:
                continue
            seen.add(id(d))
            if d.eng == eng and eng in INORDER and d.kind == 'op' and kind == 'op' and not force:
                continue
            rec.deps.append(d)
            d.needed = True
        for b in writes:
            b.w = rec
            b.r = []
        for b in reads:
            if b.w is not rec:
                if kind == 'op':
                    b.r = [x for x in b.r if not (x.kind == 'op' and x.eng == eng)]
                b.r.append(rec)
        self.prog[eng].append(rec)
        if kind == 'dma':
            self.since_barrier[eng].append(rec)
        return rec

    def op(self, eng, reads, writes, fn):
        return self._emit('op', eng, reads, writes, fn)

    def dma(self, eng, dst, src, fn, bg=False):
        r = self._emit('dma', eng, src, dst, fn)
        if bg:
            self.since_barrier[eng].remove(r)
        return r

    def barrier(self):
        marks = []
        for k in self.ENGS:
            extra = list(self.since_barrier[k])
            self.since_barrier[k] = []
            if self.prog[k]:
                extra.append(self.prog[k][-1])
            b = Buf("bar", None)
            r = self._emit('op', k, [], [b], lambda e: e.nop(), extra_deps=extra, force=True)
            r.needed = True
            marks.append(r)
        for k in self.ENGS:
            self._emit('op', k, [], [], lambda e: e.nop(), extra_deps=marks)

    def finish(self):
        nc = self.nc
        es = self.es
        self.barrier()
        csem = {k: es.enter_context(nc.semaphore(f"c_{k}")) for k in self.ENGS}
        dsem = {}
        for k in self.ENGS:
            if any(r.kind == 'dma' for r in self.prog[k]):
                dsem[k] = [es.enter_context(nc.semaphore(f"d_{k}_{i}")) for i in range(self.nds(k))]
        for k in self.ENGS:
            c = 0
            nd = 0
            for r in self.prog[k]:
                if r.kind == 'dma':
                    nds = self.nds(k)
                    slot = nd % nds
                    r.sem = dsem[k][slot]
                    r.sigval = 16 * (nd // nds + 1)
                    r.prewait = (r.sem, 16 * (nd // nds)) if nd >= nds else None
                    nd += 1
                elif r.needed:
                    c += 1
                    r.sem = csem[k]
                    r.sigval = c
        if POOL_FILTER is not None:
            self.prog['pool'] = [r for i, r in enumerate(self.prog['pool']) if POOL_FILTER(i, r)]
        block = es.enter_context(nc.Block())
        engobj = {'pe': 'tensor', 'act': 'scalar', 'dve': 'vector', 'pool': 'gpsimd', 'sp': 'sync'}
        sched = self

        def make_body(k):
            def body(e):
                known = {}
                for r in sched.prog[k]:
                    waits = {}
                    for d in r.deps:
                        key = id(d.sem)
                        if key not in waits or waits[key][1] < d.sigval:
                            waits[key] = (d.sem, d.sigval)
                    if r.prewait is not None:
                        key = id(r.prewait[0])
                        if key not in waits or waits[key][1] < r.prewait[1]:
                            waits[key] = r.prewait
                    for key, (s, v) in waits.items():
                        if known.get(key, 0) >= v:
                            continue
                        e.wait_ge(s, v)
                        known[key] = v
                    name_, a_, k_ = r.fn
                    try:
                        ins = getattr(e, name_)(*a_, **k_)
                    except Exception:
                        print("FAILED INSTR", k, name_, {kk: (getattr(vv, 'shape', vv)) for kk, vv in k_.items()})
                        raise
                    if r.kind == 'dma':
                        ins.then_inc(r.sem, 16)
                    elif r.needed:
                        ins.then_inc(r.sem, 1)
            return body

        for k in self.ENGS:
            if self.prog[k]:
                getattr(block, engobj[k])(make_body(k))


def build_program(T, L, dbg=()):
    NT = T // 128
    ST = min(1024, T)
    NST = T // ST
    TPS = ST // 128
    NBLK = (2 * T) // BLK + NEXP
    NSLOT = NBLK * BLK

    nc = bass.Bass("TRN2", target_bir_lowering=False)
    S = Sched(nc)

    def din(name, shape, dt=F32):
        return nc.dram_tensor(name, shape, dt, kind="ExternalInput").ap()

    def dscr(name, shape, dt=F32):
        kind = "ExternalOutput" if name in dbg else "Internal"
        return nc.dram_tensor(name, shape, dt, kind=kind).ap()

    x_in = din("x", [T, D])
    c_in = din("c", [1, D])
    ada_w = din("ada_w", [L, D, 6 * D])
    ada_b = din("ada_b", [L, 6 * D])
    mix_g = din("mix_norm_g", [L, D])
    in_w = din("in_w", [L, D, 8224])
    dfw_w = din("decay_fw_w", [L, 16, KDIM])
    dfw_b = din("decay_fw_b", [L, KDIM])
    dbw_w = din("decay_bw_w", [L, 16, KDIM])
    dbw_b = din("decay_bw_b", [L, KDIM])
    gla_g = din("gla_norm_g", [L, 256])
    pool_w = din("pool_w", [L, 4, 256, 256])
    pool_b = din("pool_b", [L, PDIM])
    pool_s = din("pool_scale", [L, PDIM])
    bra_w = din("branch_a_w", [L, VDIM, D])
    brb_w = din("branch_b_w", [L, PDIM, D])
    out_w = din("out_w", [L, D, D])
    ffn_g = din("ffn_norm_g", [L, D])
    rc_w = din("router_coarse_w", [L, D, 8])
    rc_b = din("router_coarse_b", [L, 8])
    rf_w = din("router_fine_w", [L, D, NEXP])
    rf_b = din("router_fine_b", [L, NEXP])
    ew13 = din("ew13", [L * NEXP * 128 * 4, 2048])
    ew2h = din("ew2h", [L * NEXP * 128 * 2, 2048])
    fin_g = din("final_norm_g", [1, D])
    cst = din("consts", [128, 6 * 128 + 8])
    cmask = din("cmask", [128, 2 * 512])
    invcnt = din("invcnt", [4, T])
    y_out = nc.dram_tensor("y", [T, D], F32, kind="ExternalOutput").ap()

    X0 = dscr("X0", [T, D])
    X1 = dscr("X1", [T, D])
    MOD = dscr("MOD", [6, D])
    QT = dscr("QT", [KDIM, T])
    KT = dscr("KT", [KDIM, T])
    LRF = dscr("LRF", [16, T])
    LRB = dscr("LRB", [16, T])
    UT = dscr("UT", [PDIM, T])
    GAT = dscr("GAT", [D, T])
    GBT = dscr("GBT", [D, T])
    KTOK = dscr("KTOK", [T, KDIM])
    VV = dscr("VV", [T, VDIM], BF16)
    RR = dscr("RR", [T, VDIM])
    OF = dscr("OF", [T, VDIM])
    OB = dscr("OB", [T, VDIM])
    H2B = dscr("H2B", [T, D], BF16)
    XS = dscr("XS", [NSLOT, D], BF16)
    YSH = [dscr("YS0", [NSLOT, D // 2]), dscr("YS1", [NSLOT, D // 2])]
    BEXP = dscr("BEXP", [1, NBLK], I32)
    EW13B = dscr("EW13B", [NEXP * 128, 8192], BF16)
    EW2B = dscr("EW2B", [NEXP * 128, 4096], BF16)

    B_ = {}
    for nm, ap in [("x", x_in), ("c", c_in), ("ada_w", ada_w), ("ada_b", ada_b), ("mix_g", mix_g), ("in_w", in_w),
                   ("dfw_w", dfw_w), ("dfw_b", dfw_b), ("dbw_w", dbw_w), ("dbw_b", dbw_b), ("gla_g", gla_g),
                   ("pool_w", pool_w), ("pool_b", pool_b), ("pool_s", pool_s), ("bra_w", bra_w), ("brb_w", brb_w),
                   ("out_w", out_w), ("ffn_g", ffn_g), ("rc_w", rc_w), ("rc_b", rc_b), ("rf_w", rf_w), ("rf_b", rf_b),
                   ("ew13", ew13), ("ew2h", ew2h), ("fin_g", fin_g), ("cst", cst), ("cmask", cmask),
                   ("invcnt", invcnt), ("y", y_out), ("X0", X0), ("X1", X1), ("MOD", MOD), ("QT", QT), ("KT", KT),
                   ("LRF", LRF), ("LRB", LRB), ("UT", UT), ("GAT", GAT), ("GBT", GBT), ("KTOK", KTOK), ("VV", VV),
                   ("RR", RR), ("OF", OF), ("OB", OB), ("H2B", H2B), ("XS", XS), ("YS", YSH[0]), ("BEXP", BEXP), ("EW13B", EW13B), ("EW2B", EW2B)]:
        B_[nm] = S.dram(nm, ap)

    with ExitStack() as es:
        S.es = es
        PF = [S.ps(f"pf{i}", [128, 512], F32) for i in range(5)]
        PB = [S.ps(f"pb{i}", [128, 1024], BF16) for i in range(3)]
        pfi = [0]

        def next_pf():
            pfi[0] = (pfi[0] + 1) % 5
            return PF[pfi[0]]

        CST = S.sb("cst", [128, 6 * 128 + 8], F32)
        CMK = S.sb("cmask", [128, 1024], F32)
        IDB = S.sb("identb", [128, 128], BF16)
        ONESB = S.sb("onesb", [128, 128], BF16)
        S.dma('sp', CST, B_["cst"], lambda e: e.dma_start(out=CST.t[:], in_=cst[:, :]))
        S.dma('sp', CMK, B_["cmask"], lambda e: e.dma_start(out=CMK.t[:], in_=cmask[:, :]))
        S.op('act', [CST], [IDB], lambda e: e.activation(out=IDB.t[:], in_=CST.t[:, 0:128], func=AF.Copy))
        S.op('act', [CST], [ONESB], lambda e: e.activation(out=ONESB.t[:], in_=CST.t[:, 640:768], func=AF.Copy))
        ident_f = CST.t[:, 0:128]
        TRI = {('f', 'A'): CST.t[:, 128:256], ('f', 'B'): CST.t[:, 256:384],
               ('b', 'A'): CST.t[:, 384:512], ('b', 'B'): CST.t[:, 512:640]}
        ones_f = CST.t[:, 640:768]

        CROW = S.sb("crow", [1, D], F32)
        SCT = S.sb("scT", [128, NKC, 2], BF16)
        S.dma('sp', CROW, B_["c"], lambda e: e.dma_start(out=CROW.t[:], in_=c_in[:, :]))
        S.op('act', [CROW], [CROW], lambda e: e.activation(out=CROW.t[:], in_=CROW.t[:], func=AF.Silu))
        pcol = next_pf()
        for kc in range(NKC):
            S.op('pe', [CROW, CST], [pcol], lambda e, kc=kc: e.matmul(
                pcol.t[:, 2 * kc:2 * kc + 2], lhsT=CROW.t[0:1, kc * 128:(kc + 1) * 128], rhs=ones_f[0:1, 0:2],
                start=True, stop=True))
        S.op('dve', [pcol], [SCT], lambda e: e.tensor_copy(out=SCT.t[:].rearrange("p k t -> p (k t)"), in_=pcol.t[:, 0:2 * NKC]))

        def row_to_col(es_, name, row_ap_fn, n, srcbuf):
            nch = n // 128
            ROW = S.sb(name + "_row", [1, n], F32, es_)
            COL = S.sb(name + "_col", [128, nch], F32, es_)
            S.dma('sp', ROW, srcbuf, lambda e: e.dma_start(out=ROW.t[:], in_=row_ap_fn()))
            pc = next_pf()
            for ci in range(nch):
                S.op('pe', [ROW, CST], [pc], lambda e, ci=ci: e.matmul(
                    pc.t[:, 2 * ci:2 * ci + 2], lhsT=ROW.t[0:1, ci * 128:(ci + 1) * 128], rhs=ones_f[0:1, 0:2],
                    start=True, stop=True))
            S.op('dve', [pc], [COL], lambda e: e.tensor_copy(
                out=COL.t[:], in_=pc.t[:, 0:2 * nch].rearrange("p (c t) -> p c t", t=2)[:, :, 0]))
            return COL

        def rms_alloc(es_, width, nseg, tag):
            return (S.sb(tag + "_ss", [128, nseg], F32, es_), S.sb(tag + "_junk", [128, width], F32, es_))

        def rms_rstd(bufs, xt, width, nseg):
            SS, JUNK = bufs
            for sgi in range(nseg):
                S.op('act', [xt], [JUNK, SS], lambda e, sgi=sgi: e.activation(
                    out=JUNK.t[:], in_=xt.t[:, sgi * width:(sgi + 1) * width], func=AF.Square,
                    accum_out=SS.t[:, sgi:sgi + 1]))
            S.op('dve', [SS], [SS], lambda e: e.tensor_scalar(out=SS.t[:], in0=SS.t[:], scalar1=1.0 / width,
                                                             scalar2=EPS, op0=ALU.mult, op1=ALU.add))
            S.op('act', [SS], [SS], lambda e: e.activation(out=SS.t[:], in_=SS.t[:], func=AF.Sqrt))
            S.op('dve', [SS], [SS], lambda e: e.reciprocal(out=SS.t[:], in_=SS.t[:]))
            return SS

        Xcur, Xnxt = B_["x"], B_["X1"]
        xcur_ap, xnxt_ap = x_in, X1

        for l in range(L):
            with ExitStack() as pes:
                ABROW = S.sb("abrow", [1, 6 * D], F32, pes)
                MROW = S.sb("mrow", [1, 6 * D], F32, pes)
                WB = [S.sb(f"wbA{i}", [128, NKC, 512], BF16, pes) for i in range(2)]
                S.dma('sp', ABROW, B_["ada_b"], lambda e: e.dma_start(out=ABROW.t[:], in_=ada_b[l:l + 1, :]))
                for gi in range(24):
                    wb = WB[gi % 2]
                    S.dma('pool', wb, B_["ada_w"], lambda e, gi=gi, wb=wb: e.dma_start(
                        out=wb.t[:], in_=ada_w[l, :, gi * 512:(gi + 1) * 512].rearrange("(k p) f -> p k f", p=128)))
                    pr = next_pf()
                    for kc in range(NKC):
                        S.op('pe', [SCT, wb], [pr], lambda e, kc=kc, wb=wb, pr=pr: e.matmul(
                            pr.t[0:1, :], lhsT=SCT.t[:, kc, 0:1], rhs=wb.t[:, kc, :], start=(kc == 0), stop=(kc == NKC - 1)))
                    S.op('dve', [pr, ABROW], [MROW], lambda e, gi=gi, pr=pr: e.tensor_tensor(
                        out=MROW.t[0:1, gi * 512:(gi + 1) * 512], in0=pr.t[0:1, :], in1=ABROW.t[0:1, gi * 512:(gi + 1) * 512],
                        op=ALU.add))
                S.dma('sp', B_["MOD"], MROW, lambda e: e.dma_start(out=MOD.rearrange("(o i) d -> o (i d)", o=1), in_=MROW.t[:]))
            S.barrier()

            with ExitStack() as pes:
                A1 = S.sb("a1bc", [128, D], F32, pes)
                B1 = S.sb("b1bc", [128, D], F32, pes)
                S.dma('sp', A1, B_["MOD"], lambda e: e.dma_start(out=A1.t[:], in_=MOD[1:2, :].to_broadcast([128, D])))
                S.dma('sp', B1, B_["mix_g"], lambda e: e.dma_start(out=B1.t[:], in_=mix_g[l:l + 1, :].to_broadcast([128, D])))
                S.op('dve', [A1, B1], [A1], lambda e: e.scalar_tensor_tensor(out=A1.t[:], in0=A1.t[:], scalar=1.0, in1=B1.t[:],
                                                                           op0=ALU.add, op1=ALU.mult))
                S.dma('sp', B1, B_["MOD"], lambda e: e.dma_start(out=B1.t[:], in_=MOD[0:1, :].to_broadcast([128, D])))
                HT = S.sb("hT", [128, NKC, ST], BF16, pes)
                XT_ = [S.sb(f"xt{i}", [128, D], F32, pes) for i in range(2)]
                HB = S.sb("hb", [128, D], BF16, pes)
                RMSB = rms_alloc(pes, D, 1, "n1")
                WB = [S.sb(f"wbB{i}", [128, NKC, 512], BF16, pes) for i in range(2)]
                STG = [S.sb(f"stg{i}", [128, 512], F32, pes) for i in range(2)]
                STGB = [S.sb(f"stgb{i}", [128, 512], BF16, pes) for i in range(2)]
                for st in range(NST):
                    t0 = st * ST
                    for tt in range(TPS):
                        xt = XT_[tt % 2]
                        r0 = t0 + tt * 128
                        S.dma('sp', xt, Xcur, lambda e, xt=xt, r0=r0: e.dma_start(out=xt.t[:], in_=xcur_ap[r0:r0 + 128, :]))
                        RS = rms_rstd(RMSB, xt, D, 1)
                        S.op('dve', [xt, RS, A1], [xt], lambda e, xt=xt, RS=RS: e.scalar_tensor_tensor(
                            out=xt.t[:], in0=xt.t[:], scalar=RS.t[:, 0:1], in1=A1.t[:], op0=ALU.mult, op1=ALU.mult))
                        S.op('dve', [xt, B1], [HB], lambda e, xt=xt: e.tensor_tensor(out=HB.t[:], in0=xt.t[:], in1=B1.t[:], op=ALU.add))
                        for half in range(2):
                            pb = PB[half]
                            for j in range(8):
                                kc = half * 8 + j
                                S.op('pe', [HB, IDB], [pb], lambda e, pb=pb, j=j, kc=kc: e.transpose(
                                    pb.t[:, j * 128:(j + 1) * 128], HB.t[:, kc * 128:(kc + 1) * 128], IDB.t[:]))
                            eng = 'act' if half == 0 else 'dve'
                            if eng == 'act':
                                S.op('act', [pb], [HT], lambda e, pb=pb, half=half, tt=tt: e.activation(
                                    out=HT.t[:, half * 8:(half + 1) * 8, tt * 128:(tt + 1) * 128],
                                    in_=pb.t[:].rearrange("p (k t) -> p k t", t=128), func=AF.Copy))
                            else:
                                S.op('dve', [pb], [HT], lambda e, pb=pb, half=half, tt=tt: e.tensor_copy(
                                    out=HT.t[:, half * 8:(half + 1) * 8, tt * 128:(tt + 1) * 128],
                                    in_=pb.t[:].rearrange("p (k t) -> p k t", t=128)))
                    gcount = [0]

                    def load_w(c0, ncols):
                        wb = WB[gcount[0] % 2]
                        gcount[0] += 1
                        S.dma('pool', wb, B_["in_w"], lambda e, wb=wb: e.dma_start(
                            out=wb.t[:, :, 0:ncols], in_=in_w[l, :, c0:c0 + ncols].rearrange("(k p) f -> p k f", p=128)))
                        return wb

                    def form_f(wb, wc0, m, dst_buf, dst_ap, row0):
                        for nt in range(ST // 512):
                            pp = next_pf()
                            for kc in range(NKC):
                                S.op('pe', [wb, HT], [pp], lambda e, pp=pp, kc=kc, nt=nt: e.matmul(
                                    pp.t[0:m, :], lhsT=wb.t[:, kc, wc0:wc0 + m], rhs=HT.t[:, kc, nt * 512:(nt + 1) * 512],
                                    start=(kc == 0), stop=(kc == NKC - 1)))
                            sg = STG[nt % 2]
                            S.op('act', [pp], [sg], lambda e, pp=pp, sg=sg: e.activation(out=sg.t[0:m, :], in_=pp.t[0:m, :], func=AF.Copy))
                            S.dma('sp', dst_buf, sg, lambda e, sg=sg, nt=nt: e.dma_start(
                                out=dst_ap[row0:row0 + m, t0 + nt * 512:t0 + (nt + 1) * 512], in_=sg.t[0:m, :]))

                    def form_t(wb, dst_buf, dst_ap, col0, bf=False):
                        for tt in range(TPS):
                            pp = next_pf()
                            for kc in range(NKC):
                                S.op('pe', [wb, HT], [pp], lambda e, pp=pp, kc=kc, tt=tt: e.matmul(
                                    pp.t[:, :], lhsT=HT.t[:, kc, tt * 128:(tt + 1) * 128], rhs=wb.t[:, kc, :],
                                    start=(kc == 0), stop=(kc == NKC - 1)))
                            sg = (STGB if bf else STG)[tt % 2]
                            S.op('dve', [pp], [sg], lambda e, pp=pp, sg=sg: e.tensor_copy(out=sg.t[:], in_=pp.t[:]))
                            S.dma('sp', dst_buf, sg, lambda e, sg=sg, tt=tt: e.dma_start(
                                out=dst_ap[t0 + tt * 128:t0 + (tt + 1) * 128, col0:col0 + 512], in_=sg.t[:]))

                    wb = load_w(0, 512)
                    for mt in range(4):
                        form_f(wb, mt * 128, 128, B_["QT"], QT, mt * 128)
                    wb = load_w(512, 512)
                    for mt in range(4):
                        form_f(wb, mt * 128, 128, B_["KT"], KT, mt * 128)
                    form_t(wb, B_["KTOK"], KTOK, 0)
                    for g2 in range(2):
                        wb = load_w(1024 + g2 * 512, 512)
                        form_t(wb, B_["VV"], VV, g2 * 512, bf=True)
                    for g2 in range(2):
                        wb = load_w(2048 + g2 * 512, 512)
                        form_t(wb, B_["RR"], RR, g2 * 512)
                    wb = load_w(3072, 32)
                    form_f(wb, 0, 16, B_["LRF"], LRF, 0)
                    form_f(wb, 16, 16, B_["LRB"], LRB, 0)
                    for g2 in range(2):
                        wb = load_w(3104 + g2 * 512, 512)
                        for mt in range(4):
                            form_f(wb, mt * 128, 128, B_["UT"], UT, g2 * 512 + mt * 128)
                    for g2 in range(4):
                        wb = load_w(4128 + g2 * 512, 512)
                        for mt in range(4):
                            form_f(wb, mt * 128, 128, B_["GAT"], GAT, g2 * 512 + mt * 128)
                    for g2 in range(4):
                        wb = load_w(6176 + g2 * 512, 512)
                        for mt in range(4):
                            form_f(wb, mt * 128, 128, B_["GBT"], GBT, g2 * 512 + mt * 128)
            S.barrier()

            for i in (range(8) if PRECAST else []):
                S.dma('pool', B_["EW13B"], B_["ew13"], lambda e, i=i: e.dma_start(
                    out=EW13B.rearrange("r (q f) -> (r q) f", q=4)[i * 4096:(i + 1) * 4096, :], in_=ew13[l * NEXP * 512 + i * 4096:l * NEXP * 512 + (i + 1) * 4096, :]), bg=True)
            for i in (range(4) if PRECAST else []):
                S.dma('pool', B_["EW2B"], B_["ew2h"], lambda e, i=i: e.dma_start(
                    out=EW2B.rearrange("r (k f) -> (r k) f", k=2)[i * 4096:(i + 1) * 4096, :], in_=ew2h[l * NEXP * 256 + i * 4096:l * NEXP * 256 + (i + 1) * 4096, :]), bg=True)
            with ExitStack() as pes:
                WD = {}
                for dname, wap, bap, wbuf, bbuf in (('f', dfw_w, dfw_b, B_["dfw_w"], B_["dfw_b"]),
                                                    ('b', dbw_w, dbw_b, B_["dbw_w"], B_["dbw_b"])):
                    wd = S.sb("wd" + dname, [17, KDIM], F32, pes)
                    S.dma('sp', wd, wbuf, lambda e, wd=wd, wap=wap: e.dma_start(out=wd.t[0:16, :], in_=wap[l, :, :]))
                    S.dma('sp', wd, bbuf, lambda e, wd=wd, bap=bap: e.dma_start(out=wd.t[16:17, :], in_=bap[l:l + 1, :]))
                    WD[dname] = wd
                def gla_dir(dname):
                    LR = [S.sb(f"lr{i}", [32, 128], F32, pes) for i in range(2)]
                    for i in range(2):
                        S.op('dve', [], [LR[i]], lambda e, i=i: e.memset(LR[i].t[:], 1.0))
                    QTt = [S.sb(f"qt{i}", [128, 4, 128], F32, pes) for i in range(2)]
                    KTt = [S.sb(f"kt{i}", [128, 4, 128], F32, pes) for i in range(2)]
                    KK = [S.sb(f"kk{i}", [128, KDIM], F32, pes) for i in range(2)]
                    VT = [S.sb(f"vt{i}", [128, VDIM], BF16, pes) for i in range(2)]
                    SP_ = S.sb("sp", [128, KDIM], F32, pes)
                    E1 = S.sb("e1", [128, 4, 128], F32, pes)
                    E2 = S.sb("e2", [128, 4, 128], F32, pes)
                    E3 = S.sb("e3", [128, KDIM], F32, pes)
                    QD = S.sb("qd", [128, 4, 128], BF16, pes)
                    KI = S.sb("ki", [128, 4, 128], BF16, pes)
                    KE = S.sb("ke", [128, KDIM], BF16, pes)
                    ATT = S.sb("att", [128, 4, 128], BF16, pes)
                    OSB = S.sb("osb", [128, VDIM], F32, pes)
                    SST = S.sb("sst", [128, VDIM], F32, pes)
                    SBF = S.sb("sbf", [128, VDIM], BF16, pes)
                    lrsrc_buf, lrsrc = (B_["LRF"], LRF) if dname == 'f' else (B_["LRB"], LRB)
                    odst_buf, odst = (B_["OF"], OF) if dname == 'f' else (B_["OB"], OB)
                    mask_ap = CMK.t[:, 0:512] if dname == 'f' else CMK.t[:, 512:1024]
                    deccol = 127 if dname == 'f' else 0
                    wd = WD[dname]
                    S.op('dve', [], [SST], lambda e: e.memset(SST.t[:], 0.0))
                    S.op('dve', [], [SBF], lambda e: e.memset(SBF.t[:], 0.0))
                    order = list(range(NT)) if dname == 'f' else list(range(NT - 1, -1, -1))
                    for ci, ch in enumerate(order):
                        c0 = ch * 128
                        lr, qt, kt, kk, vt = LR[ci % 2], QTt[ci % 2], KTt[ci % 2], KK[ci % 2], VT[ci % 2]
                        S.dma('sp', lr, lrsrc_buf, lambda e, lr=lr, c0=c0, lrsrc=lrsrc: e.dma_start(out=lr.t[0:16, :], in_=lrsrc[:, c0:c0 + 128]))
                        S.dma('sp', qt, B_["QT"], lambda e, qt=qt, c0=c0: e.dma_start(
                            out=qt.t[:], in_=QT[:, c0:c0 + 128].rearrange("(h p) t -> p h t", p=128)))
                        S.dma('sp', kt, B_["KT"], lambda e, kt=kt, c0=c0: e.dma_start(
                            out=kt.t[:], in_=KT[:, c0:c0 + 128].rearrange("(h p) t -> p h t", p=128)))
                        S.dma('sp', kk, B_["KTOK"], lambda e, kk=kk, c0=c0: e.dma_start(out=kk.t[:], in_=KTOK[c0:c0 + 128, :]))
                        S.dma('sp', vt, B_["VV"], lambda e, vt=vt, c0=c0: e.dma_start(out=vt.t[:], in_=VV[c0:c0 + 128, :]))
                        pz = next_pf()
                        S.op('pe', [lr, wd], [pz], lambda e, pz=pz, lr=lr, wd=wd: e.matmul(
                            pz.t[:, :], lhsT=lr.t[0:17, :], rhs=wd.t[0:17, :], start=True, stop=True))
                        yield
                        S.op('act', [pz], [SP_], lambda e, pz=pz: e.activation(out=SP_.t[:], in_=pz.t[:], func=AF.Exp, scale=-1.0))
                        S.op('act', [SP_], [SP_], lambda e: e.activation(out=SP_.t[:], in_=SP_.t[:], func=AF.Ln, bias=1.0))
                        pc = next_pf()
                        for h in range(4):
                            S.op('pe', [SP_, CST], [pc], lambda e, pc=pc, h=h, dname=dname: e.matmul(
                                pc.t[:, h * 128:(h + 1) * 128], lhsT=SP_.t[:, h * 128:(h + 1) * 128], rhs=TRI[(dname, 'A')],
                                start=True, stop=True))
                        pr = next_pf()
                        S.op('pe', [SP_, CST], [pr], lambda e, pr=pr, dname=dname: e.matmul(
                            pr.t[:, :], lhsT=TRI[(dname, 'B')], rhs=SP_.t[:, :], start=True, stop=True))
                        yield
                        S.op('act', [pc], [E1], lambda e, pc=pc: e.activation(out=E1.t[:].rearrange("p h t -> p (h t)"), in_=pc.t[:], func=AF.Exp))
                        S.op('act', [pc], [E2], lambda e, pc=pc: e.activation(out=E2.t[:].rearrange("p h t -> p (h t)"), in_=pc.t[:], func=AF.Exp, scale=-1.0))
                        S.op('act', [pr], [E3], lambda e, pr=pr: e.activation(out=E3.t[:], in_=pr.t[:], func=AF.Exp))
                        S.op('dve', [qt, E1], [QD], lambda e, qt=qt: e.scalar_tensor_tensor(
                            out=QD.t[:].rearrange("p h t -> p (h t)"), in0=qt.t[:].rearrange("p h t -> p (h t)"), scalar=float(128 ** -0.5),
                            in1=E1.t[:].rearrange("p h t -> p (h t)"), op0=ALU.mult, op1=ALU.mult))
                        S.op('dve', [kt, E2], [KI], lambda e, kt=kt: e.tensor_tensor(
                            out=KI.t[:].rearrange("p h t -> p (h t)"), in0=kt.t[:].rearrange("p h t -> p (h t)"),
                            in1=E2.t[:].rearrange("p h t -> p (h t)"), op=ALU.mult))
                        S.op('dve', [kk, E3], [KE], lambda e, kk=kk: e.tensor_tensor(out=KE.t[:], in0=kk.t[:], in1=E3.t[:], op=ALU.mult))
                        pa = next_pf()
                        for h in range(4):
                            S.op('pe', [KI, QD], [pa], lambda e, pa=pa, h=h: e.matmul(
                                pa.t[:, h * 128:(h + 1) * 128], lhsT=KI.t[:, h, :], rhs=QD.t[:, h, :], start=True, stop=True))
                        yield
                        S.op('dve', [pa, CMK], [ATT], lambda e, pa=pa, mask_ap=mask_ap: e.tensor_tensor(
                            out=ATT.t[:].rearrange("p h t -> p (h t)"), in0=pa.t[:], in1=mask_ap, op=ALU.mult))
                        po = [next_pf(), next_pf()]
                        for h in range(4):
                            pp = po[h // 2]
                            osl = pp.t[:, (h % 2) * 256:(h % 2 + 1) * 256]
                            S.op('pe', [ATT, vt], [pp], lambda e, osl=osl, h=h, vt=vt: e.matmul(
                                osl, lhsT=ATT.t[:, h, :], rhs=vt.t[:, h * 256:(h + 1) * 256], start=True, stop=False))
                            S.op('pe', [QD, SBF], [pp], lambda e, osl=osl, h=h: e.matmul(
                                osl, lhsT=QD.t[:, h, :], rhs=SBF.t[:, h * 256:(h + 1) * 256], start=False, stop=True))
                        yield
                        S.op('act', [po[0]], [OSB], lambda e, po=po: e.activation(out=OSB.t[:, 0:512], in_=po[0].t[:], func=AF.Copy))
                        S.op('act', [po[1]], [OSB], lambda e, po=po: e.activation(out=OSB.t[:, 512:1024], in_=po[1].t[:], func=AF.Copy))
                        S.dma('sp', odst_buf, OSB, lambda e, c0=c0, odst=odst: e.dma_start(out=odst[c0:c0 + 128, :], in_=OSB.t[:]))
                        pd = [next_pf(), next_pf()]
                        for h in range(4):
                            pp = pd[h // 2]
                            S.op('pe', [KE, vt], [pp], lambda e, pp=pp, h=h, vt=vt: e.matmul(
                                pp.t[:, (h % 2) * 256:(h % 2 + 1) * 256], lhsT=KE.t[:, h * 128:(h + 1) * 128],
                                rhs=vt.t[:, h * 256:(h + 1) * 256], start=True, stop=True))
                        yield
                        for h in range(4):
                            pp = pd[h // 2]
                            S.op('dve', [SST, E1, pp], [SST], lambda e, pp=pp, h=h, deccol=deccol: e.scalar_tensor_tensor(
                                out=SST.t[:, h * 256:(h + 1) * 256], in0=SST.t[:, h * 256:(h + 1) * 256],
                                scalar=E1.t[:, h, deccol:deccol + 1], in1=pp.t[:, (h % 2) * 256:(h % 2 + 1) * 256],
                                op0=ALU.mult, op1=ALU.add))
                        S.op('act', [SST], [SBF], lambda e: e.activation(out=SBF.t[:], in_=SST.t[:], func=AF.Copy))

                gens = [gla_dir('f'), gla_dir('b')]
                while gens:
                    for g_ in list(gens):
                        try:
                            next(g_)
                        except StopIteration:
                            gens.remove(g_)
            S.barrier()

            with ExitStack() as pes:
                GN = S.sb("gnbc", [128, 256], F32, pes)
                S.dma('sp', GN, B_["gla_g"], lambda e: e.dma_start(out=GN.t[:], in_=gla_g[l:l + 1, :].to_broadcast([128, 256])))
                G1BC = S.sb("g1bc", [128, D], F32, pes)
                S.dma('sp', G1BC, B_["MOD"], lambda e: e.dma_start(out=G1BC.t[:], in_=MOD[2:3, :].to_broadcast([128, D])))
                PBC = row_to_col(pes, "poolb", lambda: pool_b[l:l + 1, :], PDIM, B_["pool_b"])
                PSC = row_to_col(pes, "pools", lambda: pool_s[l:l + 1, :], PDIM, B_["pool_s"])
                PW = S.sb("poolw", [128, 8, 256], BF16, pes)
                S.dma('pool', PW, B_["pool_w"], lambda e: e.dma_start(
                    out=PW.t[:], in_=pool_w[l].rearrange("g (k p) o -> p (g k) o", p=128)))
                RMSD = rms_alloc(pes, 256, 4, "gn")
                GT = S.sb("gT", [128, 8, ST], BF16, pes)
                YPT = S.sb("ypT", [128, 8, ST], BF16, pes)
                MT = S.sb("mT", [128, NKC, ST], BF16, pes)
                OFt2 = [S.sb(f"oft{i}", [128, VDIM], F32, pes) for i in range(2)]
                OBt2 = [S.sb(f"obt{i}", [128, VDIM], F32, pes) for i in range(2)]
                Rt2 = [S.sb(f"rt{i}", [128, VDIM], F32, pes) for i in range(2)]
                GBt_2 = [S.sb(f"gbt{i}", [128, VDIM], BF16, pes) for i in range(2)]
                UP = S.sb("upad", [128, ST + 16], F32, pes)
                WA = S.sb("wina", [128, ST + 16], F32, pes)
                WBb = S.sb("winb", [128, ST + 16], F32, pes)
                ICN = S.sb("icn", [128, ST], F32, pes)
                PTb = S.sb("ptb", [128, 2, ST], BF16, pes)
                WBF = [S.sb(f"wbF{i}", [128, NKC, 512], BF16, pes) for i in range(2)]
                GAt = [S.sb(f"gat{i}", [128, 512], F32, pes) for i in range(2)]
                GBt2 = [S.sb(f"gbt2{i}", [128, 512], F32, pes) for i in range(2)]
                YAs = S.sb("yas", [128, 512], F32, pes)
                XTF = [S.sb(f"xtf{i}", [128, 512], F32, pes) for i in range(2)]
                for st in range(NST):
                    t0 = st * ST
                    for tt in range(TPS):
                        r0 = t0 + tt * 128
                        OFt, OBt, Rt, GBt = OFt2[tt % 2], OBt2[tt % 2], Rt2[tt % 2], GBt_2[tt % 2]
                        S.dma('sp', OFt, B_["OF"], lambda e, r0=r0: e.dma_start(out=OFt.t[:], in_=OF[r0:r0 + 128, :]))
                        S.dma('sp', OBt, B_["OB"], lambda e, r0=r0: e.dma_start(out=OBt.t[:], in_=OB[r0:r0 + 128, :]))
                        S.dma('sp', Rt, B_["RR"], lambda e, r0=r0: e.dma_start(out=Rt.t[:], in_=RR[r0:r0 + 128, :]))
                        S.op('dve', [OFt, OBt], [OFt], lambda e: e.tensor_tensor(out=OFt.t[:], in0=OFt.t[:], in1=OBt.t[:], op=ALU.add))
                        S.op('act', [Rt], [Rt], lambda e: e.activation(out=Rt.t[:], in_=Rt.t[:], func=AF.Silu))
                        RS = rms_rstd(RMSD, OFt, 256, 4)
                        for h in range(4):
                            S.op('dve', [OFt, RS, GN], [OFt], lambda e, h=h, RS=RS: e.scalar_tensor_tensor(
                                out=OFt.t[:, h * 256:(h + 1) * 256], in0=OFt.t[:, h * 256:(h + 1) * 256], scalar=RS.t[:, h:h + 1],
                                in1=GN.t[:], op0=ALU.mult, op1=ALU.mult))
                        S.op('dve', [OFt, Rt], [GBt], lambda e: e.tensor_tensor(out=GBt.t[:], in0=OFt.t[:], in1=Rt.t[:], op=ALU.mult))
                        pb = PB[tt % 2]
                        for j in range(8):
                            S.op('pe', [GBt, IDB], [pb], lambda e, pb=pb, j=j: e.transpose(
                                pb.t[:, j * 128:(j + 1) * 128], GBt.t[:, j * 128:(j + 1) * 128], IDB.t[:]))
                        S.op('act', [pb], [GT], lambda e, pb=pb, tt=tt: e.activation(
                            out=GT.t[:, :, tt * 128:(tt + 1) * 128], in_=pb.t[:].rearrange("p (k t) -> p k t", t=128), func=AF.Copy))
                    for g in range(4):
                        w = (2, 4, 8, 16)[g]
                        S.dma('sp', ICN, B_["invcnt"], lambda e, g=g: e.dma_start(
                            out=ICN.t[:], in_=invcnt[g:g + 1, t0:t0 + ST].to_broadcast([128, ST])))
                        for kc in range(2):
                            ch0 = (g * 2 + kc) * 128
                            S.op('dve', [], [UP], lambda e: e.memset(UP.t[:], 0.0))
                            lo = max(t0 - 8, 0)
                            hi = min(t0 + ST + 8, T)
                            S.dma('sp', UP, B_["UT"], lambda e, ch0=ch0, lo=lo, hi=hi: e.dma_start(
                                out=UP.t[:, lo - (t0 - 8):hi - (t0 - 8)], in_=UT[ch0:ch0 + 128, lo:hi]))
                            W_ = ST + 16
                            S.op('dve', [UP], [WA], lambda e: e.tensor_tensor(out=WA.t[:, 1:W_], in0=UP.t[:, 0:W_ - 1], in1=UP.t[:, 1:W_], op=ALU.add))
                            cur, oth = WA, WBb
                            lo_v, hi_v = 1, W_
                            sh = 1
                            while sh * 2 < w:
                                nlo, nhi = lo_v + sh, hi_v - sh
                                S.op('dve', [cur], [oth], lambda e, cur=cur, oth=oth, sh=sh, nlo=nlo, nhi=nhi: e.tensor_tensor(
                                    out=oth.t[:, nlo:nhi], in0=cur.t[:, nlo - sh:nhi - sh], in1=cur.t[:, nlo + sh:nhi + sh], op=ALU.add))
                                cur, oth = oth, cur
                                lo_v, hi_v = nlo, nhi
                                sh *= 2
                            assert lo_v <= 8 and hi_v >= ST + 8
                            S.op('dve', [cur, ICN], [oth], lambda e, cur=cur, oth=oth: e.tensor_tensor(
                                out=oth.t[:, 8:8 + ST], in0=cur.t[:, 8:8 + ST], in1=ICN.t[:], op=ALU.mult))
                            S.op('dve', [oth, UP], [PTb], lambda e, oth=oth, kc=kc: e.tensor_tensor(
                                out=PTb.t[:, kc, :], in0=oth.t[:, 8:8 + ST], in1=UP.t[:, 8:8 + ST], op=ALU.subtract))
                        for mo in range(2):
                            for nt in range(ST // 512):
                                pp = next_pf()
                                for kc in range(2):
                                    S.op('pe', [PW, PTb], [pp], lambda e, pp=pp, kc=kc, g=g, mo=mo, nt=nt: e.matmul(
                                        pp.t[:, :], lhsT=PW.t[:, g * 2 + kc, mo * 128:(mo + 1) * 128],
                                        rhs=PTb.t[:, kc, nt * 512:(nt + 1) * 512], start=(kc == 0), stop=(kc == 1)))
                                cc = g * 2 + mo
                                S.op('dve', [pp, PBC, PSC], [YPT], lambda e, pp=pp, cc=cc, nt=nt: e.tensor_scalar(
                                    out=YPT.t[:, cc, nt * 512:(nt + 1) * 512], in0=pp.t[:, :], scalar1=PBC.t[:, cc:cc + 1],
                                    scalar2=PSC.t[:, cc:cc + 1], op0=ALU.add, op1=ALU.mult))
                    for cg in range(4):
                        wa = WBF[cg % 2]
                        wbb = wa
                        S.dma('pool', wa, B_["bra_w"], lambda e, wa=wa, cg=cg: e.dma_start(
                            out=wa.t[:, 0:8, :], in_=bra_w[l, :, cg * 512:(cg + 1) * 512].rearrange("(k p) f -> p k f", p=128)))
                        S.dma('pool', wbb, B_["brb_w"], lambda e, wbb=wbb, cg=cg: e.dma_start(
                            out=wbb.t[:, 8:16, :], in_=brb_w[l, :, cg * 512:(cg + 1) * 512].rearrange("(k p) f -> p k f", p=128)))
                        for mt in range(4):
                            oc = cg * 4 + mt
                            for nt in range(ST // 512):
                                gat = GAt[nt % 2]
                                gbt = GBt2[nt % 2]
                                S.dma('sp', gat, B_["GAT"], lambda e, gat=gat, oc=oc, nt=nt: e.dma_start(
                                    out=gat.t[:], in_=GAT[oc * 128:(oc + 1) * 128, t0 + nt * 512:t0 + (nt + 1) * 512]))
                                S.dma('sp', gbt, B_["GBT"], lambda e, gbt=gbt, oc=oc, nt=nt: e.dma_start(
                                    out=gbt.t[:], in_=GBT[oc * 128:(oc + 1) * 128, t0 + nt * 512:t0 + (nt + 1) * 512]))
                                S.op('act', [gat], [gat], lambda e, gat=gat: e.activation(out=gat.t[:], in_=gat.t[:], func=AF.Sigmoid))
                                S.op('act', [gbt], [gbt], lambda e, gbt=gbt: e.activation(out=gbt.t[:], in_=gbt.t[:], func=AF.Sigmoid))
                                pa = next_pf()
                                for kc in range(8):
                                    S.op('pe', [wa, GT], [pa], lambda e, pa=pa, kc=kc, mt=mt, nt=nt, wa=wa: e.matmul(
                                        pa.t[:, :], lhsT=wa.t[:, kc, mt * 128:(mt + 1) * 128], rhs=GT.t[:, kc, nt * 512:(nt + 1) * 512],
                                        start=(kc == 0), stop=(kc == 7)))
                                pb2 = next_pf()
                                for kc in range(8):
                                    S.op('pe', [wbb, YPT], [pb2], lambda e, pb2=pb2, kc=kc, mt=mt, nt=nt, wbb=wbb: e.matmul(
                                        pb2.t[:, :], lhsT=wbb.t[:, 8 + kc, mt * 128:(mt + 1) * 128], rhs=YPT.t[:, kc, nt * 512:(nt + 1) * 512],
                                        start=(kc == 0), stop=(kc == 7)))
                                S.op('dve', [pa, gat], [YAs], lambda e, pa=pa, gat=gat: e.tensor_tensor(out=YAs.t[:], in0=pa.t[:], in1=gat.t[:], op=ALU.mult))
                                S.op('dve', [pb2, gbt], [gbt], lambda e, pb2=pb2, gbt=gbt: e.tensor_tensor(out=gbt.t[:], in0=pb2.t[:], in1=gbt.t[:], op=ALU.mult))
                                S.op('dve', [YAs, gbt], [MT], lambda e, gbt=gbt, oc=oc, nt=nt: e.tensor_tensor(
                                    out=MT.t[:, oc, nt * 512:(nt + 1) * 512], in0=YAs.t[:], in1=gbt.t[:], op=ALU.add))
                    for ng in range(4):
                        wo = WBF[ng % 2]
                        S.dma('pool', wo, B_["out_w"], lambda e, wo=wo, ng=ng: e.dma_start(
                            out=wo.t[:], in_=out_w[l, :, ng * 512:(ng + 1) * 512].rearrange("(k p) f -> p k f", p=128)))
                        for tt in range(TPS):
                            r0 = t0 + tt * 128
                            xtf = XTF[tt % 2]
                            S.dma('sp', xtf, Xcur, lambda e, xtf=xtf, r0=r0, ng=ng: e.dma_start(
                                out=xtf.t[:], in_=xcur_ap[r0:r0 + 128, ng * 512:(ng + 1) * 512]))
                            pp = next_pf()
                            for kc in range(NKC):
                                S.op('pe', [MT, wo], [pp], lambda e, pp=pp, kc=kc, tt=tt, wo=wo: e.matmul(
                                    pp.t[:, :], lhsT=MT.t[:, kc, tt * 128:(tt + 1) * 128], rhs=wo.t[:, kc, :],
                                    start=(kc == 0), stop=(kc == NKC - 1)))
                            S.op('dve', [pp, G1BC], [YAs], lambda e, pp=pp, ng=ng: e.tensor_tensor(
                                out=YAs.t[:], in0=pp.t[:], in1=G1BC.t[:, ng * 512:(ng + 1) * 512], op=ALU.mult))
                            S.op('dve', [YAs, xtf], [xtf], lambda e, xtf=xtf: e.tensor_tensor(out=xtf.t[:], in0=YAs.t[:], in1=xtf.t[:], op=ALU.add))
                            S.dma('sp', Xnxt, xtf, lambda e, xtf=xtf, r0=r0, ng=ng: e.dma_start(
                                out=xnxt_ap[r0:r0 + 128, ng * 512:(ng + 1) * 512], in_=xtf.t[:]))
            S.barrier()
            Xcur, Xnxt = Xnxt, (B_["X0"] if Xnxt is B_["X1"] else B_["X1"])
            xcur_ap, xnxt_ap = xnxt_ap, (X0 if xnxt_ap is X1 else X1)

            with ExitStack() as pes:
                A2 = S.sb("a2bc", [128, D], F32, pes)
                B2 = S.sb("b2bc", [128, D], F32, pes)
                S.dma('sp', A2, B_["MOD"], lambda e: e.dma_start(out=A2.t[:], in_=MOD[4:5, :].to_broadcast([128, D])))
                S.dma('sp', B2, B_["ffn_g"], lambda e: e.dma_start(out=B2.t[:], in_=ffn_g[l:l + 1, :].to_broadcast([128, D])))
                S.op('dve', [A2, B2], [A2], lambda e: e.scalar_tensor_tensor(out=A2.t[:], in0=A2.t[:], scalar=1.0, in1=B2.t[:],
                                                                           op0=ALU.add, op1=ALU.mult))
                S.dma('sp', B2, B_["MOD"], lambda e: e.dma_start(out=B2.t[:], in_=MOD[3:4, :].to_broadcast([128, D])))
                WR = S.sb("wr", [128, NKC, 72], F32, pes)
                S.dma('sp', WR, B_["rc_w"], lambda e: e.dma_start(out=WR.t[:, :, 0:8], in_=rc_w[l].rearrange("(k p) f -> p k f", p=128)))
                S.dma('sp', WR, B_["rf_w"], lambda e: e.dma_start(out=WR.t[:, :, 8:72], in_=rf_w[l].rearrange("(k p) f -> p k f", p=128)))
                RB = S.sb("rbias", [128, 72], F32, pes)
                S.dma('sp', RB, B_["rc_b"], lambda e: e.dma_start(out=RB.t[:, 0:8], in_=rc_b[l:l + 1, :].to_broadcast([128, 8])))
                S.dma('sp', RB, B_["rf_b"], lambda e: e.dma_start(out=RB.t[:, 8:72], in_=rf_b[l:l + 1, :].to_broadcast([128, 64])))
                OH1 = S.sb("oh1", [128, NT, NEXP], BF16, pes)
                OH2 = S.sb("oh2", [128, NT, NEXP], BF16, pes)
                LOC = S.sb("loc", [128, NT, 2], F32, pes)
                WGT = S.sb("wgt", [128, NT, 2], F32, pes)
                DST = S.sb("dst", [128, NT, 2], I32, pes)
                BASE = S.sb("base", [128, NEXP], F32, pes)
                S.op('dve', [], [BASE], lambda e: e.memset(BASE.t[:], 0.0))
                XG = [S.sb(f"xg{i}", [128, D], F32, pes) for i in range(2)]
                H2 = S.sb("h2", [128, D], F32, pes)
                RMSG = (S.sb("n2_ss", [128, 1], F32, pes), H2)
                H2b = S.sb("h2b", [128, D], BF16, pes)
                H2T = S.sb("h2T", [128, NKC, 128], F32, pes)
                LG = S.sb("lg", [128, 72], F32, pes)
                OHE = S.sb("ohe", [128, NEXP], F32, pes)
                MM_ = S.sb("mm", [128, NEXP], F32, pes)
                M2 = S.sb("m2", [128, NEXP], F32, pes)
                SM = S.sb("sm", [128, 8], F32, pes)
                JK = S.sb("jk", [128, NEXP], F32, pes)
                AA = S.sb("aa", [128, NEXP], F32, pes)
                def g1_load(tt):
                    xt_ = XG[tt % 2]
                    S.dma('sp', xt_, Xcur, lambda e, xt_=xt_, tt=tt: e.dma_start(out=xt_.t[:], in_=xcur_ap[tt * 128:(tt + 1) * 128, :]))
                g1_load(0)
                for tt in range(NT):
                    r0 = tt * 128
                    xt = XG[tt % 2]
                    if tt + 1 < NT:
                        g1_load(tt + 1)
                    RS = rms_rstd(RMSG, xt, D, 1)
                    S.op('dve', [xt, RS, A2], [H2], lambda e, xt=xt, RS=RS: e.scalar_tensor_tensor(
                        out=H2.t[:], in0=xt.t[:], scalar=RS.t[:, 0:1], in1=A2.t[:], op0=ALU.mult, op1=ALU.mult))
                    S.op('dve', [H2, B2], [H2], lambda e: e.tensor_tensor(out=H2.t[:], in0=H2.t[:], in1=B2.t[:], op=ALU.add))
                    S.op('act', [H2], [H2b], lambda e: e.activation(out=H2b.t[:], in_=H2.t[:], func=AF.Copy))
                    S.dma('sp', B_["H2B"], H2b, lambda e, r0=r0: e.dma_start(out=H2B[r0:r0 + 128, :], in_=H2b.t[:]))
                    for q4 in range(4):
                        pt = next_pf()
                        for j in range(4):
                            kc = q4 * 4 + j
                            S.op('pe', [H2, CST], [pt], lambda e, pt=pt, j=j, kc=kc: e.transpose(
                                pt.t[:, j * 128:(j + 1) * 128], H2.t[:, kc * 128:(kc + 1) * 128], ident_f))
                        S.op('act', [pt], [H2T], lambda e, pt=pt, q4=q4: e.activation(
                            out=H2T.t[:, q4 * 4:(q4 + 1) * 4, :], in_=pt.t[:].rearrange("p (k t) -> p k t", t=128), func=AF.Copy))
                    pl = next_pf()
                    for kc in range(NKC):
                        S.op('pe', [H2T, WR], [pl], lambda e, pl=pl, kc=kc: e.matmul(
                            pl.t[:, 0:72], lhsT=H2T.t[:, kc, :], rhs=WR.t[:, kc, :], start=(kc == 0), stop=(kc == NKC - 1)))
                    S.op('dve', [pl, RB], [LG], lambda e, pl=pl: e.tensor_tensor(out=LG.t[:], in0=pl.t[:, 0:72], in1=RB.t[:], op=ALU.add))
                    S.op('dve', [LG], [SM], lambda e: e.tensor_reduce(out=SM.t[:, 0:1], in_=LG.t[:, 0:8], axis=mybir.AxisListType.X, op=ALU.max))
                    S.op('dve', [SM], [SM], lambda e: e.tensor_scalar(out=SM.t[:, 1:2], in0=SM.t[:, 0:1], scalar1=-1.0, scalar2=None, op0=ALU.mult))
                    S.op('act', [LG, SM], [JK, SM], lambda e: e.activation(out=JK.t[:, 0:8], in_=LG.t[:, 0:8], func=AF.Exp,
                                                                          bias=SM.t[:, 1:2], accum_out=SM.t[:, 2:3]))
                    S.op('dve', [SM], [SM], lambda e: e.reciprocal(out=SM.t[:, 3:4], in_=SM.t[:, 2:3]))
                    S.op('dve', [LG, SM], [JK], lambda e: e.tensor_scalar(out=JK.t[:, 8:16], in0=LG.t[:, 0:8], scalar1=SM.t[:, 0:1], scalar2=None, op0=ALU.is_ge))
                    for g in range(8):
                        S.op('dve', [JK, CST], [OHE], lambda e, g=g: e.tensor_scalar(
                            out=OHE.t[:, g * 8:(g + 1) * 8], in0=ones_f[:, 0:8], scalar1=JK.t[:, 8 + g:9 + g], scalar2=None, op0=ALU.mult))
                    S.op('dve', [LG, OHE], [MM_], lambda e: e.tensor_tensor(out=MM_.t[:], in0=LG.t[:, 8:72], in1=OHE.t[:], op=ALU.mult))
                    S.op('dve', [OHE], [JK], lambda e: e.tensor_scalar(out=JK.t[:], in0=OHE.t[:], scalar1=-1.0, scalar2=BIGNEG, op0=ALU.add, op1=ALU.mult))
                    S.op('dve', [MM_, JK], [MM_], lambda e: e.tensor_tensor(out=MM_.t[:], in0=MM_.t[:], in1=JK.t[:], op=ALU.add))
                    S.op('dve', [MM_], [SM], lambda e: e.tensor_reduce(out=SM.t[:, 4:5], in_=MM_.t[:], axis=mybir.AxisListType.X, op=ALU.max))
                    S.op('dve', [MM_, SM], [OH1], lambda e, tt=tt: e.tensor_scalar(out=OH1.t[:, tt, :], in0=MM_.t[:], scalar1=SM.t[:, 4:5], scalar2=None, op0=ALU.is_ge))
                    S.op('dve', [OH1, MM_], [M2], lambda e, tt=tt: e.scalar_tensor_tensor(out=M2.t[:], in0=OH1.t[:, tt, :], scalar=-BIGNEG, in1=MM_.t[:],
                                                                                          op0=ALU.mult, op1=ALU.add))
                    S.op('dve', [M2], [SM], lambda e: e.tensor_reduce(out=SM.t[:, 5:6], in_=M2.t[:], axis=mybir.AxisListType.X, op=ALU.max))
                    S.op('dve', [M2, SM], [OH2], lambda e, tt=tt: e.tensor_scalar(out=OH2.t[:, tt, :], in0=M2.t[:], scalar1=SM.t[:, 5:6], scalar2=None, op0=ALU.is_ge))
                    S.op('dve', [SM], [SM], lambda e: e.tensor_tensor(out=SM.t[:, 6:7], in0=SM.t[:, 5:6], in1=SM.t[:, 4:5], op=ALU.subtract))
                    S.op('act', [SM], [SM], lambda e: e.activation(out=SM.t[:, 6:7], in_=SM.t[:, 6:7], func=AF.Exp))
                    S.op('dve', [SM], [SM], lambda e: e.tensor_scalar(out=SM.t[:, 7:8], in0=SM.t[:, 6:7], scalar1=1.0, scalar2=None, op0=ALU.add))
                    S.op('dve', [SM], [SM], lambda e: e.reciprocal(out=SM.t[:, 7:8], in_=SM.t[:, 7:8]))
                    S.op('dve', [SM], [WGT], lambda e, tt=tt: e.tensor_tensor(out=WGT.t[:, tt, 0:1], in0=SM.t[:, 7:8], in1=SM.t[:, 3:4], op=ALU.mult))
                    S.op('dve', [SM, WGT], [WGT], lambda e, tt=tt: e.tensor_tensor(out=WGT.t[:, tt, 1:2], in0=WGT.t[:, tt, 0:1], in1=SM.t[:, 6:7], op=ALU.mult))
                    S.op('dve', [OH1, OH2], [AA], lambda e, tt=tt: e.tensor_tensor(out=AA.t[:], in0=OH1.t[:, tt, :], in1=OH2.t[:, tt, :], op=ALU.add))
                    pq = next_pf()
                    S.op('pe', [AA, CST], [pq], lambda e, pq=pq: e.matmul(pq.t[:, 0:64], lhsT=TRI[('b', 'B')], rhs=AA.t[:], start=True, stop=True))
                    S.op('pe', [AA, CST], [pq], lambda e, pq=pq: e.matmul(pq.t[:, 64:128], lhsT=ones_f, rhs=AA.t[:], start=True, stop=True))
                    S.op('dve', [pq, BASE], [JK], lambda e, pq=pq: e.scalar_tensor_tensor(out=JK.t[:], in0=pq.t[:, 0:64], scalar=-16.0, in1=BASE.t[:],
                                                                                         op0=ALU.mult, op1=ALU.add))
                    S.op('dve', [JK, OH1], [M2, LOC], lambda e, tt=tt: e.scalar_tensor_tensor(
                        out=M2.t[:], in0=JK.t[:], scalar=1.0, in1=OH1.t[:, tt, :], op0=ALU.mult, op1=ALU.mult, accum_out=LOC.t[:, tt, 0:1]))
                    S.op('dve', [JK, OH2], [M2, LOC], lambda e, tt=tt: e.scalar_tensor_tensor(
                        out=M2.t[:], in0=JK.t[:], scalar=1.0, in1=OH2.t[:, tt, :], op0=ALU.mult, op1=ALU.mult, accum_out=LOC.t[:, tt, 1:2]))
                    S.op('dve', [pq, BASE], [BASE], lambda e, pq=pq: e.tensor_tensor(out=BASE.t[:], in0=BASE.t[:], in1=pq.t[:, 64:128], op=ALU.add))
                NBK = S.sb("nbk", [128, NEXP], F32, pes)
                CS0 = S.sb("cs0", [128, NEXP], F32, pes)
                CS1 = S.sb("cs1", [128, NEXP], F32, pes)
                PST = S.sb("pst", [128, NEXP], F32, pes)
                BEX = S.sb("bex", [128, NBLK], F32, pes)
                BEXI = S.sb("bexi", [128, NBLK], I32, pes)
                S.op('dve', [], [NBK], lambda e: e.memset(NBK.t[:], 0.0))
                for k in range(T // BLK):
                    S.op('dve', [BASE, NBK], [NBK], lambda e, k=k: e.scalar_tensor_tensor(
                        out=NBK.t[:], in0=BASE.t[:], scalar=float(k * BLK), in1=NBK.t[:], op0=ALU.is_gt, op1=ALU.add))
                S.op('dve', [NBK], [CS0], lambda e: e.tensor_copy(out=CS0.t[:], in_=NBK.t[:]))
                cur, oth = CS0, CS1
                sh = 1
                while sh < NEXP:
                    S.op('dve', [cur], [oth], lambda e, cur=cur, oth=oth: e.tensor_copy(out=oth.t[:], in_=cur.t[:]))
                    S.op('dve', [cur], [oth], lambda e, cur=cur, oth=oth, sh=sh: e.tensor_tensor(
                        out=oth.t[:, sh:NEXP], in0=cur.t[:, sh:NEXP], in1=cur.t[:, 0:NEXP - sh], op=ALU.add))
                    cur, oth = oth, cur
                    sh *= 2
                PEND = cur
                S.op('dve', [PEND, NBK], [PST], lambda e, PEND=PEND: e.tensor_tensor(out=PST.t[:], in0=PEND.t[:], in1=NBK.t[:], op=ALU.subtract))
                for b in range(NBLK):
                    S.op('dve', [PEND], [JK, BEX], lambda e, b=b, PEND=PEND: e.tensor_scalar(
                        out=JK.t[:], in0=PEND.t[:], scalar1=float(b), scalar2=0.0, op0=ALU.is_le, op1=ALU.add, accum_out=BEX.t[:, b:b + 1]))
                SKF = S.sb("skf", [128, NBLK], F32, pes)
                S.op('dve', [BEX], [SKF], lambda e: e.tensor_scalar(out=SKF.t[:], in0=BEX.t[:], scalar1=float(NEXP) - 0.5, scalar2=(1.0e7 if SKIP_OOB else 0.0),
                                                                     op0=ALU.is_ge, op1=ALU.mult))
                S.op('dve', [BEX], [BEX], lambda e: e.tensor_scalar(out=BEX.t[:], in0=BEX.t[:], scalar1=float(NEXP - 1), scalar2=None, op0=ALU.min))
                S.op('dve', [BEX], [BEXI], lambda e: e.tensor_copy(out=BEXI.t[:], in_=BEX.t[:]))
                for tt in range(NT):
                    for r in range(2):
                        OH = OH1 if r == 0 else OH2
                        S.op('dve', [PST, OH], [M2, SM], lambda e, tt=tt, OH=OH: e.scalar_tensor_tensor(
                            out=M2.t[:], in0=PST.t[:], scalar=float(BLK), in1=OH.t[:, tt, :], op0=ALU.mult, op1=ALU.mult, accum_out=SM.t[:, 0:1]))
                        S.op('dve', [SM, LOC], [SM], lambda e, tt=tt, r=r: e.tensor_tensor(out=SM.t[:, 1:2], in0=SM.t[:, 0:1], in1=LOC.t[:, tt, r:r + 1], op=ALU.add))
                        S.op('dve', [SM], [DST], lambda e, tt=tt, r=r: e.tensor_copy(out=DST.t[:, tt, r:r + 1], in_=SM.t[:, 1:2]))
                XB = [S.sb(f"xb{i}", [128, D], BF16, pes) for i in range(2)]
                for tt in range(NT):
                    r0 = tt * 128
                    xb = XB[tt % 2]
                    S.dma('sp', xb, B_["H2B"], lambda e, xb=xb, r0=r0: e.dma_start(out=xb.t[:], in_=H2B[r0:r0 + 128, :]))
                    for r in range(2):
                        S.dma('pool', B_["XS"], [xb, DST], lambda e, xb=xb, tt=tt, r=r: e.indirect_dma_start(
                            out=XS[:, :], out_offset=bass.IndirectOffsetOnAxis(ap=DST.t[:, tt, r:r + 1], axis=0),
                            in_=xb.t[:], in_offset=None))
                W13 = [S.sb(f"w13{i}", [128, NKC, 512], BF16, pes) for i in range(3)]
                W2 = [S.sb(f"w2{i}", [128, 2, D], BF16, pes) for i in range(3)]
                XBT = S.sb("xbT", [128, NKC, 128], BF16, pes)
                ACTf = S.sb("actf", [128, FF], F32, pes)
                ACTb = S.sb("actb", [128, FF], BF16, pes)
                ACTT = S.sb("actT", [128, 2, 128], BF16, pes)
                YSB = [S.sb(f"ysb{i}", [128, D], F32, pes) for i in range(2)]
                E5 = S.sb("e512", [128, NBLK], F32, pes)
                E2G = S.sb("e256", [128, NBLK], F32, pes)
                IDXA = S.sb("idxa", [128, NBLK, 4], I32, pes)
                IDXB = S.sb("idxb", [128, NBLK, 2], I32, pes)
                KPL = S.sb("kpl", [128, 8], F32, pes)
                S.op('dve', [CST], [KPL], lambda e: e.tensor_scalar(out=KPL.t[:, 0:4], in0=CST.t[:, 768:772], scalar1=(0.25 if PRECAST else float(l * NEXP * 512)), scalar2=None, op0=(ALU.mult if PRECAST else ALU.add)))
                S.op('dve', [CST], [KPL], lambda e: e.tensor_scalar(out=KPL.t[:, 4:6], in0=CST.t[:, 772:774], scalar1=(0.0 if PRECAST else float(l * NEXP * 256)), scalar2=None, op0=ALU.add))
                S.op('dve', [BEX, SKF], [E5], lambda e: e.scalar_tensor_tensor(out=E5.t[:], in0=BEX.t[:], scalar=(128.0 if PRECAST else 512.0), in1=SKF.t[:], op0=ALU.mult, op1=ALU.add))
                S.op('dve', [BEX, SKF], [E2G], lambda e: e.scalar_tensor_tensor(out=E2G.t[:], in0=BEX.t[:], scalar=256.0, in1=SKF.t[:], op0=ALU.mult, op1=ALU.add))
                for b in range(NBLK):
                    S.op('dve', [E5, KPL], [IDXA], lambda e, b=b: e.scalar_tensor_tensor(
                        out=IDXA.t[:, b, :], in0=ones_f[:, 0:4], scalar=E5.t[:, b:b + 1], in1=KPL.t[:, 0:4], op0=ALU.mult, op1=ALU.add))
                    S.op('dve', [E2G, KPL], [IDXB], lambda e, b=b: e.scalar_tensor_tensor(
                        out=IDXB.t[:, b, :], in0=ones_f[:, 0:2], scalar=E2G.t[:, b:b + 1], in1=KPL.t[:, 4:6], op0=ALU.mult, op1=ALU.add))
                XBT2 = [XBT, S.sb("xbT1", [128, NKC, 128], BF16, pes)]
                ACTf2 = [ACTf, S.sb("actf1", [128, FF], F32, pes)]
                ACTb2 = [ACTb, S.sb("actb1", [128, FF], BF16, pes)]
                ACTT2 = [ACTT, S.sb("actT1", [128, 2, 128], BF16, pes)]
                NSB = NBLK * NSUB

                def g3_load(n):
                    xb_ = XB[n % 2]
                    S.dma('sp', xb_, B_["XS"], lambda e, xb_=xb_, n=n: e.dma_start(out=xb_.t[:], in_=XS[n * 128:(n + 1) * 128, :]))

                def g3_weights(b):
                    w13_, w2_ = W13[b % 3], W2[b % 3]
                    S.dma('pool', w13_, [B_["EW13B"], IDXA], lambda e, w13_=w13_, b=b: e.indirect_dma_start(
                        out=w13_.t[:].rearrange("p k f -> p (k f)"), out_offset=None, in_=EW13B[:, :],
                        in_offset=bass.IndirectOffsetOnAxis(ap=IDXA.t[:, b, 0:1], axis=0)))
                    S.dma('pool', w2_, [B_["EW2B"], IDXA], lambda e, w2_=w2_, b=b: e.indirect_dma_start(
                        out=w2_.t[:].rearrange("p k f -> p (k f)"), out_offset=None, in_=EW2B[:, :],
                        in_offset=bass.IndirectOffsetOnAxis(ap=IDXA.t[:, b, 0:1], axis=0)))

                PHS = {}

                def stA(n):
                    xb = XB[n % 2]
                    xbt = XBT2[n % 2]
                    for half in range(2):
                        pb = PB[half]
                        for j in range(8):
                            kc = half * 8 + j
                            S.op('pe', [xb, IDB], [pb], lambda e, pb=pb, j=j, kc=kc, xb=xb: e.transpose(
                                pb.t[:, j * 128:(j + 1) * 128], xb.t[:, kc * 128:(kc + 1) * 128], IDB.t[:]))
                        if half == 0:
                            S.op('act', [pb], [xbt], lambda e, pb=pb, half=half, xbt=xbt: e.activation(
                                out=xbt.t[:, half * 8:(half + 1) * 8, :], in_=pb.t[:].rearrange("p (k t) -> p k t", t=128), func=AF.Copy))
                        else:
                            S.op('dve', [pb], [xbt], lambda e, pb=pb, half=half, xbt=xbt: e.tensor_copy(
                                out=xbt.t[:, half * 8:(half + 1) * 8, :], in_=pb.t[:].rearrange("p (k t) -> p k t", t=128)))

                def stB(n):
                    w13 = W13[(n // NSUB) % 3]
                    xbt, actf, actb = XBT2[n % 2], ACTf2[n % 2], ACTb2[n % 2]
                    ph = next_pf()
                    for kc in range(NKC):
                        S.op('pe', [xbt, w13], [ph], lambda e, ph=ph, kc=kc, w13=w13, xbt=xbt: e.matmul(
                            ph.t[:, :], lhsT=xbt.t[:, kc, :], rhs=w13.t[:, kc, :], start=(kc == 0), stop=(kc == NKC - 1)))
                    S.op('act', [ph], [actf], lambda e, ph=ph, actf=actf: e.activation(out=actf.t[:], in_=ph.t[:, 0:FF], func=AF.Silu))
                    S.op('dve', [actf, ph], [actb], lambda e, ph=ph, actf=actf, actb=actb: e.tensor_tensor(
                        out=actb.t[:], in0=actf.t[:], in1=ph.t[:, FF:2 * FF], op=ALU.mult))

                def stC(n):
                    actb, actt = ACTb2[n % 2], ACTT2[n % 2]
                    pbt = PB[2]
                    for j in range(2):
                        S.op('pe', [actb, IDB], [pbt], lambda e, j=j, pbt=pbt, actb=actb: e.transpose(
                            pbt.t[:, j * 128:(j + 1) * 128], actb.t[:, j * 128:(j + 1) * 128], IDB.t[:]))
                    S.op('act', [pbt], [actt], lambda e, pbt=pbt, actt=actt: e.activation(
                        out=actt.t[:], in_=pbt.t[:, 0:256].rearrange("p (k t) -> p k t", t=128), func=AF.Copy))

                def stD(n):
                    w2 = W2[(n // NSUB) % 3]
                    actt = ACTT2[n % 2]
                    ysb = YSB[n % 2]
                    for nt in range(4):
                        py = next_pf()
                        for kc in range(2):
                            S.op('pe', [actt, w2], [py], lambda e, py=py, kc=kc, nt=nt, w2=w2, actt=actt: e.matmul(
                                py.t[:, :], lhsT=actt.t[:, kc, :], rhs=w2.t[:, kc, nt * 512:(nt + 1) * 512], start=(kc == 0), stop=(kc == 1)))
                        if nt % 2 == 0:
                            S.op('act', [py], [ysb], lambda e, py=py, nt=nt, ysb=ysb: e.activation(out=ysb.t[:, nt * 512:(nt + 1) * 512], in_=py.t[:], func=AF.Copy))
                        else:
                            S.op('dve', [py], [ysb], lambda e, py=py, nt=nt, ysb=ysb: e.tensor_copy(out=ysb.t[:, nt * 512:(nt + 1) * 512], in_=py.t[:]))
                    for hh in range(2):
                        S.dma('sp', B_["YS"], ysb, lambda e, ysb=ysb, n=n, hh=hh: e.dma_start(
                            out=YSH[hh][n * 128:(n + 1) * 128, :], in_=ysb.t[:, hh * 1024:(hh + 1) * 1024]))

                g3_load(0)
                for b0 in range(min(3, NBLK)):
                    g3_weights(b0)
                for it in range(NSB + 3):
                    nA, nB, nC, nD = it, it - 1, it - 2, it - 3
                    if nD >= 0 and nD % NSUB == 0:
                        bD = nD // NSUB
                        if bD >= 1 and bD + 2 < NBLK:
                            g3_weights(bD + 2)
                    if nA < NSB:
                        if nA + 1 < NSB:
                            g3_load(nA + 1)
                        stA(nA)
                    if 0 <= nB < NSB:
                        stB(nB)
                    if 0 <= nC < NSB:
                        stC(nC)
                    if 0 <= nD < NSB:
                        stD(nD)
                G2BC = A2
                S.dma('sp', G2BC, B_["MOD"], lambda e: e.dma_start(out=G2BC.t[:], in_=MOD[5:6, :].to_broadcast([128, D])))
                YA = [YSB[0], H2]
                YB = [YSB[1], Buf("h2t_alias", H2T.t[:].rearrange("p k t -> p (k t)"))]

                def g4_loads(tt):
                    xt_ = XG[tt % 2]
                    S.dma('sp', xt_, Xcur, lambda e, xt_=xt_, tt=tt: e.dma_start(out=xt_.t[:], in_=xcur_ap[tt * 128:(tt + 1) * 128, :]))
                    for r, yb_ in ((0, YA[tt % 2]), (1, YB[tt % 2])):
                        for hh in range(2):
                            S.dma('pool', yb_, [B_["YS"], DST], lambda e, yb_=yb_, tt=tt, r=r, hh=hh: e.indirect_dma_start(
                                out=yb_.t[:, hh * 1024:(hh + 1) * 1024], out_offset=None, in_=YSH[hh][:, :],
                                in_offset=bass.IndirectOffsetOnAxis(ap=DST.t[:, tt, r:r + 1], axis=0)))

                g4_loads(0)
                for tt in range(NT):
                    r0 = tt * 128
                    if tt + 1 < NT:
                        g4_loads(tt + 1)
                    xt = XG[tt % 2]
                    y0, y1 = YA[tt % 2], YB[tt % 2]
                    S.op('dve', [y0, WGT], [y0], lambda e, y0=y0, tt=tt: e.tensor_scalar(out=y0.t[:], in0=y0.t[:], scalar1=WGT.t[:, tt, 0:1], scalar2=None, op0=ALU.mult))
                    S.op('dve', [y1, WGT, y0], [y0], lambda e, y0=y0, y1=y1, tt=tt: e.scalar_tensor_tensor(
                        out=y0.t[:], in0=y1.t[:], scalar=WGT.t[:, tt, 1:2], in1=y0.t[:], op0=ALU.mult, op1=ALU.add))
                    S.op('dve', [y0, G2BC], [y0], lambda e, y0=y0: e.tensor_tensor(out=y0.t[:], in0=y0.t[:], in1=G2BC.t[:], op=ALU.mult))
                    S.op('dve', [y0, xt], [xt], lambda e, y0=y0, xt=xt: e.tensor_tensor(out=xt.t[:], in0=y0.t[:], in1=xt.t[:], op=ALU.add))
                    S.dma('sp', Xnxt, xt, lambda e, xt=xt, r0=r0: e.dma_start(out=xnxt_ap[r0:r0 + 128, :], in_=xt.t[:]))
            S.barrier()
            Xcur, Xnxt = Xnxt, (B_["X0"] if Xnxt is B_["X1"] else B_["X1"])
            xcur_ap, xnxt_ap = xnxt_ap, (X0 if xnxt_ap is X1 else X1)

        with ExitStack() as pes:
            FG = S.sb("fgbc", [128, D], F32, pes)
            S.dma('sp', FG, B_["fin_g"], lambda e: e.dma_start(out=FG.t[:], in_=fin_g[0:1, :].to_broadcast([128, D])))
            XF = [S.sb(f"xf{i}", [128, D], F32, pes) for i in range(2)]
            RMSF = rms_alloc(pes, D, 1, "nf")
            def fin_load(tt):
                xt_ = XF[tt % 2]
                S.dma('sp', xt_, Xcur, lambda e, xt_=xt_, tt=tt: e.dma_start(out=xt_.t[:], in_=xcur_ap[tt * 128:(tt + 1) * 128, :]))
            fin_load(0)
            for tt in range(NT):
                r0 = tt * 128
                xt = XF[tt % 2]
                if tt + 1 < NT:
                    fin_load(tt + 1)
                RS = rms_rstd(RMSF, xt, D, 1)
                S.op('dve', [xt, RS, FG], [xt], lambda e, xt=xt, RS=RS: e.scalar_tensor_tensor(
                    out=xt.t[:], in0=xt.t[:], scalar=RS.t[:, 0:1], in1=FG.t[:], op0=ALU.mult, op1=ALU.mult))
                S.dma('sp', B_["y"], xt, lambda e, xt=xt, r0=r0: e.dma_start(out=y_out[r0:r0 + 128, :], in_=xt.t[:]))
        S.finish()
    return nc


def make_consts(T):
    i = np.arange(128)
    s = i[:, None]
    t = i[None, :]
    cst = np.zeros((128, 6 * 128 + 8), np.float32)
    for q in range(4):
        cst[:, 768 + q] = 4 * i + q
    for kc in range(2):
        cst[:, 772 + kc] = 2 * i + kc
    cst[:, 0:128] = np.eye(128, dtype=np.float32)
    f = -1.0 / 16.0
    cst[:, 128:256] = f * (s <= t)
    cst[:, 256:384] = f * (s > t)
    cst[:, 384:512] = f * (s >= t)
    cst[:, 512:640] = f * (s < t)
    cst[:, 640:768] = 1.0
    cm = np.zeros((128, 1024), np.float32)
    mf = (s <= t).astype(np.float32)
    mb = (s > t).astype(np.float32)
    for h in range(4):
        cm[:, h * 128:(h + 1) * 128] = mf
        cm[:, 512 + h * 128:512 + (h + 1) * 128] = mb
    tt = np.arange(T)
    ic = np.zeros((4, T), np.float32)
    for g, w in enumerate((2, 4, 8, 16)):
        lo = np.clip(tt - w // 2, 0, T)
        hi = np.clip(tt + w // 2, 0, T)
        ic[g] = 1.0 / (hi - lo).astype(np.float32)
    return cst, cm, ic


_PROG = {}


def run(inputs, T, L, ncores, dbg=()):
    key = (T, L, tuple(dbg))
    if key not in _PROG:
        _PROG[key] = build_program(T, L, dbg)
    nc = _PROG[key]
    cst, cm, ic = make_consts(T)
    f = lambda a: np.ascontiguousarray(np.asarray(a, dtype=np.float32))
    shared = {k: f(inputs[k]) for k in (
        "ada_w", "ada_b", "mix_norm_g", "in_w", "decay_fw_w", "decay_fw_b", "decay_bw_w", "decay_bw_b", "gla_norm_g",
        "pool_w", "pool_b", "pool_scale", "branch_a_w", "branch_b_w", "out_w", "ffn_norm_g", "router_coarse_w",
        "router_coarse_b", "router_fine_w", "router_fine_b")}
    shared = {k: v[:L] for k, v in shared.items()}
    w1 = f(inputs["expert_w1"])[:L]
    w3 = f(inputs["expert_w3"])[:L]
    w2 = f(inputs["expert_w2"])[:L]
    w13 = np.concatenate([w1, w3], axis=-1).reshape(L, NEXP, NKC, 128, 2 * FF).transpose(0, 1, 3, 2, 4)
    shared["ew13"] = np.ascontiguousarray(w13).reshape(L * NEXP * 128 * 4, 2048)
    del w13, w1, w3
    w2h = w2.reshape(L, NEXP, 2, 128, D).transpose(0, 1, 3, 2, 4)
    shared["ew2h"] = np.ascontiguousarray(w2h).reshape(L * NEXP * 128 * 2, 2048)
    del w2h, w2
    shared["final_norm_g"] = f(inputs["final_norm_g"]).reshape(1, D)
    shared["consts"] = cst
    shared["cmask"] = cm
    shared["invcnt"] = ic
    x = f(inputs["x"])
    c = f(inputs["c"])
    in_maps = []
    for b in range(ncores):
        m = dict(shared)
        m["x"] = x[b]
        m["c"] = c[b:b + 1]
        in_maps.append(m)
    res = run_bass_kernel_spmd(nc, in_maps, core_ids=list(range(ncores)), **RUN_KW)
    return res


def kernel(**inputs):
    x = np.asarray(inputs["x"])
    Bn, T, _ = x.shape
    L = np.asarray(inputs["ada_w"]).shape[0]
    res = run(inputs, T, L, Bn)
    return np.stack([np.asarray(res.results[b]["y"], dtype=np.float32) for b in range(Bn)], axis=0)
```

```python
import numpy as np
from contextlib import ExitStack
import concourse.bass as bass
import concourse.mybir as mybir
from concourse.bass_utils import run_bass_kernel_spmd

F32 = mybir.dt.float32
BF16 = mybir.dt.bfloat16
I32 = mybir.dt.int32
ALU = mybir.AluOpType
AF = mybir.ActivationFunctionType

D = 2048
NKC = 16
KDIM = 512
VDIM = 1024
PDIM = 1024
NEXP = 64
FF = 256
EPS = 1e-6
import os as _os
BLK = int(_os.environ.get('K_BLK', '256'))
NSUB = BLK // 128
SKIP_OOB = False
PRECAST = True
BIGNEG = 1.0e30
POOL_FILTER = None
import os as _os
INORDER = tuple(_os.environ.get('K_INORDER', 'pe').split(','))
RUN_KW = {}


class Buf:
    __slots__ = ("name", "t", "w", "r")

    def __init__(self, name, t):
        self.name, self.t, self.w, self.r = name, t, None, []


class Rec:
    __slots__ = ("kind", "eng", "fn", "deps", "needed", "sem", "sigval", "prewait")

    def __init__(self, kind, eng, fn):
        self.kind, self.eng, self.fn = kind, eng, fn
        self.deps = []
        self.needed = False
        self.sem = None
        self.sigval = None
        self.prewait = None


class Sched:
    ENGS = ('pe', 'act', 'dve', 'pool', 'sp')
    NDS = 8

    def __init__(self, nc):
        self.nc = nc
        self.prog = {k: [] for k in self.ENGS}
        self.es = None
        self.nbuf = 0
        self.since_barrier = {k: [] for k in self.ENGS}

    def nds(self, k):
        import os
        return int(os.environ.get("POOL_NDS", "8")) if k == 'pool' else self.NDS

    def sb(self, name, shape, dt, es=None):
        self.nbuf += 1
        t = (es or self.es).enter_context(self.nc.sbuf_tensor(f"{name}_{self.nbuf}", shape, dt))
        return Buf(name, t)

    def ps(self, name, shape, dt):
        self.nbuf += 1
        t = self.es.enter_context(self.nc.psum_tensor(f"{name}_{self.nbuf}", shape, dt))
        return Buf(name, t)

    def dram(self, name, ap):
        return Buf(name, ap)

    def _emit(self, kind, eng, reads, writes, fn, extra_deps=(), force=False):
        calls = []

        class _P:
            def __getattr__(self_, name):
                def f(*a, **k):
                    calls.append((name, a, k))
                    return None
                return f
        fn(_P())
        assert len(calls) == 1
        rec = Rec(kind, eng, calls[0])
        if not isinstance(reads, (list, tuple)):
            reads = [reads]
        if not isinstance(writes, (list, tuple)):
            writes = [writes]
        deps = list(extra_deps)
        for b in reads:
            if b.w is not None:
                deps.append(b.w)
        for b in writes:
            if b.w is not None:
                deps.append(b.w)
            deps.extend(b.r)
        seen = set()
        for d in deps:
            if id(d) in seen or d is rec:
                continue
            seen.add(id(d))
            if d.eng == eng and eng in INORDER and d.kind == 'op' and kind == 'op' and not force:
                continue
            rec.deps.append(d)
            d.needed = True
        for b in writes:
            b.w = rec
            b.r = []
        for b in reads:
            if b.w is not rec:
                if kind == 'op':
                    b.r = [x for x in b.r if not (x.kind == 'op' and x.eng == eng)]
                b.r.append(rec)
        self.prog[eng].append(rec)
        if kind == 'dma':
            self.since_barrier[eng].append(rec)
        return rec

    def op(self, eng, reads, writes, fn):
        return self._emit('op', eng, reads, writes, fn)

    def dma(self, eng, dst, src, fn, bg=False):
        r = self._emit('dma', eng, src, dst, fn)
        if bg:
            self.since_barrier[eng].remove(r)
        return r

    def barrier(self):
        marks = []
        for k in self.ENGS:
            extra = list(self.since_barrier[k])
            self.since_barrier[k] = []
            if self.prog[k]:
                extra.append(self.prog[k][-1])
            b = Buf("bar", None)
            r = self._emit('op', k, [], [b], lambda e: e.nop(), extra_deps=extra, force=True)
            r.needed = True
            marks.append(r)
        for k in self.ENGS:
            self._emit('op', k, [], [], lambda e: e.nop(), extra_deps=marks)

    def finish(self):
        nc = self.nc
        es = self.es
        self.barrier()
        csem = {k: es.enter_context(nc.semaphore(f"c_{k}")) for k in self.ENGS}
        dsem = {}
        for k in self.ENGS:
            if any(r.kind == 'dma' for r in self.prog[k]):
                dsem[k] = [es.enter_context(nc.semaphore(f"d_{k}_{i}")) for i in range(self.nds(k))]
        for k in self.ENGS:
            c = 0
            nd = 0
            for r in self.prog[k]:
                if r.kind == 'dma':
                    nds = self.nds(k)
                    slot = nd % nds
                    r.sem = dsem[k][slot]
                    r.sigval = 16 * (nd // nds + 1)
                    r.prewait = (r.sem, 16 * (nd // nds)) if nd >= nds else None
                    nd += 1
                elif r.needed:
                    c += 1
                    r.sem = csem[k]
                    r.sigval = c
        if POOL_FILTER is not None:
            self.prog['pool'] = [r for i, r in enumerate(self.prog['pool']) if POOL_FILTER(i, r)]
        block = es.enter_context(nc.Block())
        engobj = {'pe': 'tensor', 'act': 'scalar', 'dve': 'vector', 'pool': 'gpsimd', 'sp': 'sync'}
        sched = self

        def make_body(k):
            def body(e):
                known = {}
                for r in sched.prog[k]:
                    waits = {}
                    for d in r.deps:
                        key = id(d.sem)
                        if key not in waits or waits[key][1] < d.sigval:
                            waits[key] = (d.sem, d.sigval)
                    if r.prewait is not None:
                        key = id(r.prewait[0])
                        if key not in waits or waits[key][1] < r.prewait[1]:
                            waits[key] = r.prewait
                    for key, (s, v) in waits.items():
                        if known.get(key, 0) >= v:
                            continue
                        e.wait_ge(s, v)
                        known[key] = v
                    name_, a_, k_ = r.fn
                    try:
                        ins = getattr(e, name_)(*a_, **k_)
                    except Exception:
                        print("FAILED INSTR", k, name_, {kk: (getattr(vv, 'shape', vv)) for kk, vv in k_.items()})
                        raise
                    if r.kind == 'dma':
                        ins.then_inc(r.sem, 16)
                    elif r.needed:
                        ins.then_inc(r.sem, 1)
            return body

        for k in self.ENGS:
            if self.prog[k]:
                getattr(block, engobj[k])(make_body(k))


def build_program(T, L, dbg=()):
    NT = T // 128
    ST = min(1024, T)
    NST = T // ST
    TPS = ST // 128
    NBLK = (2 * T) // BLK + NEXP
    NSLOT = NBLK * BLK

    nc = bass.Bass("TRN2", target_bir_lowering=False)
    S = Sched(nc)

    def din(name, shape, dt=F32):
        return nc.dram_tensor(name, shape, dt, kind="ExternalInput").ap()

    def dscr(name, shape, dt=F32):
        kind = "ExternalOutput" if name in dbg else "Internal"
        return nc.dram_tensor(name, shape, dt, kind=kind).ap()

    x_in = din("x", [T, D])
    c_in = din("c", [1, D])
    ada_w = din("ada_w", [L, D, 6 * D])
    ada_b = din("ada_b", [L, 6 * D])
    mix_g = din("mix_norm_g", [L, D])
    in_w = din("in_w", [L, D, 8224])
    dfw_w = din("decay_fw_w", [L, 16, KDIM])
    dfw_b = din("decay_fw_b", [L, KDIM])
    dbw_w = din("decay_bw_w", [L, 16, KDIM])
    dbw_b = din("decay_bw_b", [L, KDIM])
    gla_g = din("gla_norm_g", [L, 256])
    pool_w = din("pool_w", [L, 4, 256, 256])
    pool_b = din("pool_b", [L, PDIM])
    pool_s = din("pool_scale", [L, PDIM])
    bra_w = din("branch_a_w", [L, VDIM, D])
    brb_w = din("branch_b_w", [L, PDIM, D])
    out_w = din("out_w", [L, D, D])
    ffn_g = din("ffn_norm_g", [L, D])
    rc_w = din("router_coarse_w", [L, D, 8])
    rc_b = din("router_coarse_b", [L, 8])
    rf_w = din("router_fine_w", [L, D, NEXP])
    rf_b = din("router_fine_b", [L, NEXP])
    ew13 = din("ew13", [L * NEXP * 128 * 4, 2048])
    ew2h = din("ew2h", [L * NEXP * 128 * 2, 2048])
    fin_g = din("final_norm_g", [1, D])
    cst = din("consts", [128, 6 * 128 + 8])
    cmask = din("cmask", [128, 2 * 512])
    invcnt = din("invcnt", [4, T])
    y_out = nc.dram_tensor("y", [T, D], F32, kind="ExternalOutput").ap()

    X0 = dscr("X0", [T, D])
    X1 = dscr("X1", [T, D])
    MOD = dscr("MOD", [6, D])
    QT = dscr("QT", [KDIM, T])
    KT = dscr("KT", [KDIM, T])
    LRF = dscr("LRF", [16, T])
    LRB = dscr("LRB", [16, T])
    UT = dscr("UT", [PDIM, T])
    GAT = dscr("GAT", [D, T])
    GBT = dscr("GBT", [D, T])
    KTOK = dscr("KTOK", [T, KDIM])
    VV = dscr("VV", [T, VDIM], BF16)
    RR = dscr("RR", [T, VDIM])
    OF = dscr("OF", [T, VDIM])
    OB = dscr("OB", [T, VDIM])
    H2B = dscr("H2B", [T, D], BF16)
    XS = dscr("XS", [NSLOT, D], BF16)
    YSH = [dscr("YS0", [NSLOT, D // 2]), dscr("YS1", [NSLOT, D // 2])]
    BEXP = dscr("BEXP", [1, NBLK], I32)
    EW13B = dscr("EW13B", [NEXP * 128, 8192], BF16)
    EW2B = dscr("EW2B", [NEXP * 128, 4096], BF16)

    B_ = {}
    for nm, ap in [("x", x_in), ("c", c_in), ("ada_w", ada_w), ("ada_b", ada_b), ("mix_g", mix_g), ("in_w", in_w),
                   ("dfw_w", dfw_w), ("dfw_b", dfw_b), ("dbw_w", dbw_w), ("dbw_b", dbw_b), ("gla_g", gla_g),
                   ("pool_w", pool_w), ("pool_b", pool_b), ("pool_s", pool_s), ("bra_w", bra_w), ("brb_w", brb_w),
                   ("out_w", out_w), ("ffn_g", ffn_g), ("rc_w", rc_w), ("rc_b", rc_b), ("rf_w", rf_w), ("rf_b", rf_b),
                   ("ew13", ew13), ("ew2h", ew2h), ("fin_g", fin_g), ("cst", cst), ("cmask", cmask),
                   ("invcnt", invcnt), ("y", y_out), ("X0", X0), ("X1", X1), ("MOD", MOD), ("QT", QT), ("KT", KT),
                   ("LRF", LRF), ("LRB", LRB), ("UT", UT), ("GAT", GAT), ("GBT", GBT), ("KTOK", KTOK), ("VV", VV),
                   ("RR", RR), ("OF", OF), ("OB", OB), ("H2B", H2B), ("XS", XS), ("YS", YSH[0]), ("BEXP", BEXP), ("EW13B", EW13B), ("EW2B", EW2B)]:
        B_[nm] = S.dram(nm, ap)

    with ExitStack() as es:
        S.es = es
        PF = [S.ps(f"pf{i}", [128, 512], F32) for i in range(5)]
        PB = [S.ps(f"pb{i}", [128, 1024], BF16) for i in range(3)]
        pfi = [0]

        def next_pf():
            pfi[0] = (pfi[0] + 1) % 5
            return PF[pfi[0]]

        CST = S.sb("cst", [128, 6 * 128 + 8], F32)
        CMK = S.sb("cmask", [128, 1024], F32)
        IDB = S.sb("identb", [128, 128], BF16)
        ONESB = S.sb("onesb", [128, 128], BF16)
        S.dma('sp', CST, B_["cst"], lambda e: e.dma_start(out=CST.t[:], in_=cst[:, :]))
        S.dma('sp', CMK, B_["cmask"], lambda e: e.dma_start(out=CMK.t[:], in_=cmask[:, :]))
        S.op('act', [CST], [IDB], lambda e: e.activation(out=IDB.t[:], in_=CST.t[:, 0:128], func=AF.Copy))
        S.op('act', [CST], [ONESB], lambda e: e.activation(out=ONESB.t[:], in_=CST.t[:, 640:768], func=AF.Copy))
        ident_f = CST.t[:, 0:128]
        TRI = {('f', 'A'): CST.t[:, 128:256], ('f', 'B'): CST.t[:, 256:384],
               ('b', 'A'): CST.t[:, 384:512], ('b', 'B'): CST.t[:, 512:640]}
        ones_f = CST.t[:, 640:768]

        CROW = S.sb("crow", [1, D], F32)
        SCT = S.sb("scT", [128, NKC, 2], BF16)
        S.dma('sp', CROW, B_["c"], lambda e: e.dma_start(out=CROW.t[:], in_=c_in[:, :]))
        S.op('act', [CROW], [CROW], lambda e: e.activation(out=CROW.t[:], in_=CROW.t[:], func=AF.Silu))
        pcol = next_pf()
        for kc in range(NKC):
            S.op('pe', [CROW, CST], [pcol], lambda e, kc=kc: e.matmul(
                pcol.t[:, 2 * kc:2 * kc + 2], lhsT=CROW.t[0:1, kc * 128:(kc + 1) * 128], rhs=ones_f[0:1, 0:2],
                start=True, stop=True))
        S.op('dve', [pcol], [SCT], lambda e: e.tensor_copy(out=SCT.t[:].rearrange("p k t -> p (k t)"), in_=pcol.t[:, 0:2 * NKC]))

        def row_to_col(es_, name, row_ap_fn, n, srcbuf):
            nch = n // 128
            ROW = S.sb(name + "_row", [1, n], F32, es_)
            COL = S.sb(name + "_col", [128, nch], F32, es_)
            S.dma('sp', ROW, srcbuf, lambda e: e.dma_start(out=ROW.t[:], in_=row_ap_fn()))
            pc = next_pf()
            for ci in range(nch):
                S.op('pe', [ROW, CST], [pc], lambda e, ci=ci: e.matmul(
                    pc.t[:, 2 * ci:2 * ci + 2], lhsT=ROW.t[0:1, ci * 128:(ci + 1) * 128], rhs=ones_f[0:1, 0:2],
                    start=True, stop=True))
            S.op('dve', [pc], [COL], lambda e: e.tensor_copy(
                out=COL.t[:], in_=pc.t[:, 0:2 * nch].rearrange("p (c t) -> p c t", t=2)[:, :, 0]))
            return COL

        def rms_alloc(es_, width, nseg, tag):
            return (S.sb(tag + "_ss", [128, nseg], F32, es_), S.sb(tag + "_junk", [128, width], F32, es_))

        def rms_rstd(bufs, xt, width, nseg):
            SS, JUNK = bufs
            for sgi in range(nseg):
                S.op('act', [xt], [JUNK, SS], lambda e, sgi=sgi: e.activation(
                    out=JUNK.t[:], in_=xt.t[:, sgi * width:(sgi + 1) * width], func=AF.Square,
                    accum_out=SS.t[:, sgi:sgi + 1]))
            S.op('dve', [SS], [SS], lambda e: e.tensor_scalar(out=SS.t[:], in0=SS.t[:], scalar1=1.0 / width,
                                                             scalar2=EPS, op0=ALU.mult, op1=ALU.add))
            S.op('act', [SS], [SS], lambda e: e.activation(out=SS.t[:], in_=SS.t[:], func=AF.Sqrt))
            S.op('dve', [SS], [SS], lambda e: e.reciprocal(out=SS.t[:], in_=SS.t[:]))
            return SS

        Xcur, Xnxt = B_["x"], B_["X1"]
        xcur_ap, xnxt_ap = x_in, X1

        for l in range(L):
            with ExitStack() as pes:
                ABROW = S.sb("abrow", [1, 6 * D], F32, pes)
                MROW = S.sb("mrow", [1, 6 * D], F32, pes)
                WB = [S.sb(f"wbA{i}", [128, NKC, 512], BF16, pes) for i in range(2)]
                S.dma('sp', ABROW, B_["ada_b"], lambda e: e.dma_start(out=ABROW.t[:], in_=ada_b[l:l + 1, :]))
                for gi in range(24):
                    wb = WB[gi % 2]
                    S.dma('pool', wb, B_["ada_w"], lambda e, gi=gi, wb=wb: e.dma_start(
                        out=wb.t[:], in_=ada_w[l, :, gi * 512:(gi + 1) * 512].rearrange("(k p) f -> p k f", p=128)))
                    pr = next_pf()
                    for kc in range(NKC):
                        S.op('pe', [SCT, wb], [pr], lambda e, kc=kc, wb=wb, pr=pr: e.matmul(
                            pr.t[0:1, :], lhsT=SCT.t[:, kc, 0:1], rhs=wb.t[:, kc, :], start=(kc == 0), stop=(kc == NKC - 1)))
                    S.op('dve', [pr, ABROW], [MROW], lambda e, gi=gi, pr=pr: e.tensor_tensor(
                        out=MROW.t[0:1, gi * 512:(gi + 1) * 512], in0=pr.t[0:1, :], in1=ABROW.t[0:1, gi * 512:(gi + 1) * 512],
                        op=ALU.add))
                S.dma('sp', B_["MOD"], MROW, lambda e: e.dma_start(out=MOD.rearrange("(o i) d -> o (i d)", o=1), in_=MROW.t[:]))
            S.barrier()

            with ExitStack() as pes:
                A1 = S.sb("a1bc", [128, D], F32, pes)
                B1 = S.sb("b1bc", [128, D], F32, pes)
                S.dma('sp', A1, B_["MOD"], lambda e: e.dma_start(out=A1.t[:], in_=MOD[1:2, :].to_broadcast([128, D])))
                S.dma('sp', B1, B_["mix_g"], lambda e: e.dma_start(out=B1.t[:], in_=mix_g[l:l + 1, :].to_broadcast([128, D])))
                S.op('dve', [A1, B1], [A1], lambda e: e.scalar_tensor_tensor(out=A1.t[:], in0=A1.t[:], scalar=1.0, in1=B1.t[:],
                                                                           op0=ALU.add, op1=ALU.mult))
                S.dma('sp', B1, B_["MOD"], lambda e: e.dma_start(out=B1.t[:], in_=MOD[0:1, :].to_broadcast([128, D])))
                HT = S.sb("hT", [128, NKC, ST], BF16, pes)
                XT_ = [S.sb(f"xt{i}", [128, D], F32, pes) for i in range(2)]
                HB = S.sb("hb", [128, D], BF16, pes)
                RMSB = rms_alloc(pes, D, 1, "n1")
                WB = [S.sb(f"wbB{i}", [128, NKC, 512], BF16, pes) for i in range(2)]
                STG = [S.sb(f"stg{i}", [128, 512], F32, pes) for i in range(2)]
                STGB = [S.sb(f"stgb{i}", [128, 512], BF16, pes) for i in range(2)]
                for st in range(NST):
                    t0 = st * ST
                    for tt in range(TPS):
                        xt = XT_[tt % 2]
                        r0 = t0 + tt * 128
                        S.dma('sp', xt, Xcur, lambda e, xt=xt, r0=r0: e.dma_start(out=xt.t[:], in_=xcur_ap[r0:r0 + 128, :]))
                        RS = rms_rstd(RMSB, xt, D, 1)
                        S.op('dve', [xt, RS, A1], [xt], lambda e, xt=xt, RS=RS: e.scalar_tensor_tensor(
                            out=xt.t[:], in0=xt.t[:], scalar=RS.t[:, 0:1], in1=A1.t[:], op0=ALU.mult, op1=ALU.mult))
                        S.op('dve', [xt, B1], [HB], lambda e, xt=xt: e.tensor_tensor(out=HB.t[:], in0=xt.t[:], in1=B1.t[:], op=ALU.add))
                        for half in range(2):
                            pb = PB[half]
                            for j in range(8):
                                kc = half * 8 + j
                                S.op('pe', [HB, IDB], [pb], lambda e, pb=pb, j=j, kc=kc: e.transpose(
                                    pb.t[:, j * 128:(j + 1) * 128], HB.t[:, kc * 128:(kc + 1) * 128], IDB.t[:]))
                            eng = 'act' if half == 0 else 'dve'
                            if eng == 'act':
                                S.op('act', [pb], [HT], lambda e, pb=pb, half=half, tt=tt: e.activation(
                                    out=HT.t[:, half * 8:(half + 1) * 8, tt * 128:(tt + 1) * 128],
                                    in_=pb.t[:].rearrange("p (k t) -> p k t", t=128), func=AF.Copy))
                            else:
                                S.op('dve', [pb], [HT], lambda e, pb=pb, half=half, tt=tt: e.tensor_copy(
                                    out=HT.t[:, half * 8:(half + 1) * 8, tt * 128:(tt + 1) * 128],
                                    in_=pb.t[:].rearrange("p (k t) -> p k t", t=128)))
                    gcount = [0]

                    def load_w(c0, ncols):
                        wb = WB[gcount[0] % 2]
                        gcount[0] += 1
                        S.dma('pool', wb, B_["in_w"], lambda e, wb=wb: e.dma_start(
                            out=wb.t[:, :, 0:ncols], in_=in_w[l, :, c0:c0 + ncols].rearrange("(k p) f -> p k f", p=128)))
                        return wb

                    def form_f(wb, wc0, m, dst_buf, dst_ap, row0):
                        for nt in range(ST // 512):
                            pp = next_pf()
                            for kc in range(NKC):
                                S.op('pe', [wb, HT], [pp], lambda e, pp=pp, kc=kc, nt=nt: e.matmul(
                                    pp.t[0:m, :], lhsT=wb.t[:, kc, wc0:wc0 + m], rhs=HT.t[:, kc, nt * 512:(nt + 1) * 512],
                                    start=(kc == 0), stop=(kc == NKC - 1)))
                            sg = STG[nt % 2]
                            S.op('act', [pp], [sg], lambda e, pp=pp, sg=sg: e.activation(out=sg.t[0:m, :], in_=pp.t[0:m, :], func=AF.Copy))
                            S.dma('sp', dst_buf, sg, lambda e, sg=sg, nt=nt: e.dma_start(
                                out=dst_ap[row0:row0 + m, t0 + nt * 512:t0 + (nt + 1) * 512], in_=sg.t[0:m, :]))

                    def form_t(wb, dst_buf, dst_ap, col0, bf=False):
                        for tt in range(TPS):
                            pp = next_pf()
                            for kc in range(NKC):
                                S.op('pe', [wb, HT], [pp], lambda e, pp=pp, kc=kc, tt=tt: e.matmul(
                                    pp.t[:, :], lhsT=HT.t[:, kc, tt * 128:(tt + 1) * 128], rhs=wb.t[:, kc, :],
                                    start=(kc == 0), stop=(kc == NKC - 1)))
                            sg = (STGB if bf else STG)[tt % 2]
                            S.op('dve', [pp], [sg], lambda e, pp=pp, sg=sg: e.tensor_copy(out=sg.t[:], in_=pp.t[:]))
                            S.dma('sp', dst_buf, sg, lambda e, sg=sg, tt=tt: e.dma_start(
                                out=dst_ap[t0 + tt * 128:t0 + (tt + 1) * 128, col0:col0 + 512], in_=sg.t[:]))

                    wb = load_w(0, 512)
                    for mt in range(4):
                        form_f(wb, mt * 128, 128, B_["QT"], QT, mt * 128)
                    wb = load_w(512, 512)
                    for mt in range(4):
                        form_f(wb, mt * 128, 128, B_["KT"], KT, mt * 128)
                    form_t(wb, B_["KTOK"], KTOK, 0)
                    for g2 in range(2):
                        wb = load_w(1024 + g2 * 512, 512)
                        form_t(wb, B_["VV"], VV, g2 * 512, bf=True)
                    for g2 in range(2):
                        wb = load_w(2048 + g2 * 512, 512)
                        form_t(wb, B_["RR"], RR, g2 * 512)
                    wb = load_w(3072, 32)
                    form_f(wb, 0, 16, B_["LRF"], LRF, 0)
                    form_f(wb, 16, 16, B_["LRB"], LRB, 0)
                    for g2 in range(2):
                        wb = load_w(3104 + g2 * 512, 512)
                        for mt in range(4):
                            form_f(wb, mt * 128, 128, B_["UT"], UT, g2 * 512 + mt * 128)
                    for g2 in range(4):
                        wb = load_w(4128 + g2 * 512, 512)
                        for mt in range(4):
                            form_f(wb, mt * 128, 128, B_["GAT"], GAT, g2 * 512 + mt * 128)
                    for g2 in range(4):
                        wb = load_w(6176 + g2 * 512, 512)
                        for mt in range(4):
                            form_f(wb, mt * 128, 128, B_["GBT"], GBT, g2 * 512 + mt * 128)
            S.barrier()

            for i in (range(8) if PRECAST else []):
                S.dma('pool', B_["EW13B"], B_["ew13"], lambda e, i=i: e.dma_start(
                    out=EW13B.rearrange("r (q f) -> (r q) f", q=4)[i * 4096:(i + 1) * 4096, :], in_=ew13[l * NEXP * 512 + i * 4096:l * NEXP * 512 + (i + 1) * 4096, :]), bg=True)
            for i in (range(4) if PRECAST else []):
                S.dma('pool', B_["EW2B"], B_["ew2h"], lambda e, i=i: e.dma_start(
                    out=EW2B.rearrange("r (k f) -> (r k) f", k=2)[i * 4096:(i + 1) * 4096, :], in_=ew2h[l * NEXP * 256 + i * 4096:l * NEXP * 256 + (i + 1) * 4096, :]), bg=True)
            with ExitStack() as pes:
                WD = {}
                for dname, wap, bap, wbuf, bbuf in (('f', dfw_w, dfw_b, B_["dfw_w"], B_["dfw_b"]),
                                                    ('b', dbw_w, dbw_b, B_["dbw_w"], B_["dbw_b"])):
                    wd = S.sb("wd" + dname, [17, KDIM], F32, pes)
                    S.dma('sp', wd, wbuf, lambda e, wd=wd, wap=wap: e.dma_start(out=wd.t[0:16, :], in_=wap[l, :, :]))
                    S.dma('sp', wd, bbuf, lambda e, wd=wd, bap=bap: e.dma_start(out=wd.t[16:17, :], in_=bap[l:l + 1, :]))
                    WD[dname] = wd
                def gla_dir(dname):
                    LR = [S.sb(f"lr{i}", [32, 128], F32, pes) for i in range(2)]
                    for i in range(2):
                        S.op('dve', [], [LR[i]], lambda e, i=i: e.memset(LR[i].t[:], 1.0))
                    QTt = [S.sb(f"qt{i}", [128, 4, 128], F32, pes) for i in range(2)]
                    KTt = [S.sb(f"kt{i}", [128, 4, 128], F32, pes) for i in range(2)]
                    KK = [S.sb(f"kk{i}", [128, KDIM], F32, pes) for i in range(2)]
                    VT = [S.sb(f"vt{i}", [128, VDIM], BF16, pes) for i in range(2)]
                    SP_ = S.sb("sp", [128, KDIM], F32, pes)
                    E1 = S.sb("e1", [128, 4, 128], F32, pes)
                    E2 = S.sb("e2", [128, 4, 128], F32, pes)
                    E3 = S.sb("e3", [128, KDIM], F32, pes)
                    QD = S.sb("qd", [128, 4, 128], BF16, pes)
                    KI = S.sb("ki", [128, 4, 128], BF16, pes)
                    KE = S.sb("ke", [128, KDIM], BF16, pes)
                    ATT = S.sb("att", [128, 4, 128], BF16, pes)
                    OSB = S.sb("osb", [128, VDIM], F32, pes)
                    SST = S.sb("sst", [128, VDIM], F32, pes)
                    SBF = S.sb("sbf", [128, VDIM], BF16, pes)
                    lrsrc_buf, lrsrc = (B_["LRF"], LRF) if dname == 'f' else (B_["LRB"], LRB)
                    odst_buf, odst = (B_["OF"], OF) if dname == 'f' else (B_["OB"], OB)
                    mask_ap = CMK.t[:, 0:512] if dname == 'f' else CMK.t[:, 512:1024]
                    deccol = 127 if dname == 'f' else 0
                    wd = WD[dname]
                    S.op('dve', [], [SST], lambda e: e.memset(SST.t[:], 0.0))
                    S.op('dve', [], [SBF], lambda e: e.memset(SBF.t[:], 0.0))
                    order = list(range(NT)) if dname == 'f' else list(range(NT - 1, -1, -1))
                    for ci, ch in enumerate(order):
                        c0 = ch * 128
                        lr, qt, kt, kk, vt = LR[ci % 2], QTt[ci % 2], KTt[ci % 2], KK[ci % 2], VT[ci % 2]
                        S.dma('sp', lr, lrsrc_buf, lambda e, lr=lr, c0=c0, lrsrc=lrsrc: e.dma_start(out=lr.t[0:16, :], in_=lrsrc[:, c0:c0 + 128]))
                        S.dma('sp', qt, B_["QT"], lambda e, qt=qt, c0=c0: e.dma_start(
                            out=qt.t[:], in_=QT[:, c0:c0 + 128].rearrange("(h p) t -> p h t", p=128)))
                        S.dma('sp', kt, B_["KT"], lambda e, kt=kt, c0=c0: e.dma_start(
                            out=kt.t[:], in_=KT[:, c0:c0 + 128].rearrange("(h p) t -> p h t", p=128)))
                        S.dma('sp', kk, B_["KTOK"], lambda e, kk=kk, c0=c0: e.dma_start(out=kk.t[:], in_=KTOK[c0:c0 + 128, :]))
                        S.dma('sp', vt, B_["VV"], lambda e, vt=vt, c0=c0: e.dma_start(out=vt.t[:], in_=VV[c0:c0 + 128, :]))
                        pz = next_pf()
                        S.op('pe', [lr, wd], [pz], lambda e, pz=pz, lr=lr, wd=wd: e.matmul(
                            pz.t[:, :], lhsT=lr.t[0:17, :], rhs=wd.t[0:17, :], start=True, stop=True))
                        yield
                        S.op('act', [pz], [SP_], lambda e, pz=pz: e.activation(out=SP_.t[:], in_=pz.t[:], func=AF.Exp, scale=-1.0))
                        S.op('act', [SP_], [SP_], lambda e: e.activation(out=SP_.t[:], in_=SP_.t[:], func=AF.Ln, bias=1.0))
                        pc = next_pf()
                        for h in range(4):
                            S.op('pe', [SP_, CST], [pc], lambda e, pc=pc, h=h, dname=dname: e.matmul(
                                pc.t[:, h * 128:(h + 1) * 128], lhsT=SP_.t[:, h * 128:(h + 1) * 128], rhs=TRI[(dname, 'A')],
                                start=True, stop=True))
                        pr = next_pf()
                        S.op('pe', [SP_, CST], [pr], lambda e, pr=pr, dname=dname: e.matmul(
                            pr.t[:, :], lhsT=TRI[(dname, 'B')], rhs=SP_.t[:, :], start=True, stop=True))
                        yield
                        S.op('act', [pc], [E1], lambda e, pc=pc: e.activation(out=E1.t[:].rearrange("p h t -> p (h t)"), in_=pc.t[:], func=AF.Exp))
                        S.op('act', [pc], [E2], lambda e, pc=pc: e.activation(out=E2.t[:].rearrange("p h t -> p (h t)"), in_=pc.t[:], func=AF.Exp, scale=-1.0))
                        S.op('act', [pr], [E3], lambda e, pr=pr: e.activation(out=E3.t[:], in_=pr.t[:], func=AF.Exp))
                        S.op('dve', [qt, E1], [QD], lambda e, qt=qt: e.scalar_tensor_tensor(
                            out=QD.t[:].rearrange("p h t -> p (h t)"), in0=qt.t[:].rearrange("p h t -> p (h t)"), scalar=float(128 ** -0.5),
                            in1=E1.t[:].rearrange("p h t -> p (h t)"), op0=ALU.mult, op1=ALU.mult))
                        S.op('dve', [kt, E2], [KI], lambda e, kt=kt: e.tensor_tensor(
                            out=KI.t[:].rearrange("p h t -> p (h t)"), in0=kt.t[:].rearrange("p h t -> p (h t)"),
                            in1=E2.t[:].rearrange("p h t -> p (h t)"), op=ALU.mult))
                        S.op('dve', [kk, E3], [KE], lambda e, kk=kk: e.tensor_tensor(out=KE.t[:], in0=kk.t[:], in1=E3.t[:], op=ALU.mult))
                        pa = next_pf()
                        for h in range(4):
                            S.op('pe', [KI, QD], [pa], lambda e, pa=pa, h=h: e.matmul(
                                pa.t[:, h * 128:(h + 1) * 128], lhsT=KI.t[:, h, :], rhs=QD.t[:, h, :], start=True, stop=True))
                        yield
                        S.op('dve', [pa, CMK], [ATT], lambda e, pa=pa, mask_ap=mask_ap: e.tensor_tensor(
                            out=ATT.t[:].rearrange("p h t -> p (h t)"), in0=pa.t[:], in1=mask_ap, op=ALU.mult))
                        po = [next_pf(), next_pf()]
                        for h in range(4):
                            pp = po[h // 2]
                            osl = pp.t[:, (h % 2) * 256:(h % 2 + 1) * 256]
                            S.op('pe', [ATT, vt], [pp], lambda e, osl=osl, h=h, vt=vt: e.matmul(
                                osl, lhsT=ATT.t[:, h, :], rhs=vt.t[:, h * 256:(h + 1) * 256], start=True, stop=False))
                            S.op('pe', [QD, SBF], [pp], lambda e, osl=osl, h=h: e.matmul(
                                osl, lhsT=QD.t[:, h, :], rhs=SBF.t[:, h * 256:(h + 1) * 256], start=False, stop=True))
                        yield
                        S.op('act', [po[0]], [OSB], lambda e, po=po: e.activation(out=OSB.t[:, 0:512], in_=po[0].t[:], func=AF.Copy))
                        S.op('act', [po[1]], [OSB], lambda e, po=po: e.activation(out=OSB.t[:, 512:1024], in_=po[1].t[:], func=AF.Copy))
                        S.dma('sp', odst_buf, OSB, lambda e, c0=c0, odst=odst: e.dma_start(out=odst[c0:c0 + 128, :], in_=OSB.t[:]))
                        pd = [next_pf(), next_pf()]
                        for h in range(4):
                            pp = pd[h // 2]
                            S.op('pe', [KE, vt], [pp], lambda e, pp=pp, h=h, vt=vt: e.matmul(
                                pp.t[:, (h % 2) * 256:(h % 2 + 1) * 256], lhsT=KE.t[:, h * 128:(h + 1) * 128],
                                rhs=vt.t[:, h * 256:(h + 1) * 256], start=True, stop=True))
                        yield
                        for h in range(4):
                            pp = pd[h // 2]
                            S.op('dve', [SST, E1, pp], [SST], lambda e, pp=pp, h=h, deccol=deccol: e.scalar_tensor_tensor(
                                out=SST.t[:, h * 256:(h + 1) * 256], in0=SST.t[:, h * 256:(h + 1) * 256],
                                scalar=E1.t[:, h, deccol:deccol + 1], in1=pp.t[:, (h % 2) * 256:(h % 2 + 1) * 256],
                                op0=ALU.mult, op1=ALU.add))
                        S.op('act', [SST], [SBF], lambda e: e.activation(out=SBF.t[:], in_=SST.t[:], func=AF.Copy))

                gens = [gla_dir('f'), gla_dir('b')]
                while gens:
                    for g_ in list(gens):
                        try:
                            next(g_)
                        except StopIteration:
                            gens.remove(g_)
            S.barrier()

            with ExitStack() as pes:
                GN = S.sb("gnbc", [128, 256], F32, pes)
                S.dma('sp', GN, B_["gla_g"], lambda e: e.dma_start(out=GN.t[:], in_=gla_g[l:l + 1, :].to_broadcast([128, 256])))
                G1BC = S.sb("g1bc", [128, D], F32, pes)
                S.dma('sp', G1BC, B_["MOD"], lambda e: e.dma_start(out=G1BC.t[:], in_=MOD[2:3, :].to_broadcast([128, D])))
                PBC = row_to_col(pes, "poolb", lambda: pool_b[l:l + 1, :], PDIM, B_["pool_b"])
                PSC = row_to_col(pes, "pools", lambda: pool_s[l:l + 1, :], PDIM, B_["pool_s"])
                PW = S.sb("poolw", [128, 8, 256], BF16, pes)
                S.dma('pool', PW, B_["pool_w"], lambda e: e.dma_start(
                    out=PW.t[:], in_=pool_w[l].rearrange("g (k p) o -> p (g k) o", p=128)))
                RMSD = rms_alloc(pes, 256, 4, "gn")
                GT = S.sb("gT", [128, 8, ST], BF16, pes)
                YPT = S.sb("ypT", [128, 8, ST], BF16, pes)
                MT = S.sb("mT", [128, NKC, ST], BF16, pes)
                OFt2 = [S.sb(f"oft{i}", [128, VDIM], F32, pes) for i in range(2)]
                OBt2 = [S.sb(f"obt{i}", [128, VDIM], F32, pes) for i in range(2)]
                Rt2 = [S.sb(f"rt{i}", [128, VDIM], F32, pes) for i in range(2)]
                GBt_2 = [S.sb(f"gbt{i}", [128, VDIM], BF16, pes) for i in range(2)]
                UP = S.sb("upad", [128, ST + 16], F32, pes)
                WA = S.sb("wina", [128, ST + 16], F32, pes)
                WBb = S.sb("winb", [128, ST + 16], F32, pes)
                ICN = S.sb("icn", [128, ST], F32, pes)
                PTb = S.sb("ptb", [128, 2, ST], BF16, pes)
                WBF = [S.sb(f"wbF{i}", [128, NKC, 512], BF16, pes) for i in range(2)]
                GAt = [S.sb(f"gat{i}", [128, 512], F32, pes) for i in range(2)]
                GBt2 = [S.sb(f"gbt2{i}", [128, 512], F32, pes) for i in range(2)]
                YAs = S.sb("yas", [128, 512], F32, pes)
                XTF = [S.sb(f"xtf{i}", [128, 512], F32, pes) for i in range(2)]
                for st in range(NST):
                    t0 = st * ST
                    for tt in range(TPS):
                        r0 = t0 + tt * 128
                        OFt, OBt, Rt, GBt = OFt2[tt % 2], OBt2[tt % 2], Rt2[tt % 2], GBt_2[tt % 2]
                        S.dma('sp', OFt, B_["OF"], lambda e, r0=r0: e.dma_start(out=OFt.t[:], in_=OF[r0:r0 + 128, :]))
                        S.dma('sp', OBt, B_["OB"], lambda e, r0=r0: e.dma_start(out=OBt.t[:], in_=OB[r0:r0 + 128, :]))
                        S.dma('sp', Rt, B_["RR"], lambda e, r0=r0: e.dma_start(out=Rt.t[:], in_=RR[r0:r0 + 128, :]))
                        S.op('dve', [OFt, OBt], [OFt], lambda e: e.tensor_tensor(out=OFt.t[:], in0=OFt.t[:], in1=OBt.t[:], op=ALU.add))
                        S.op('act', [Rt], [Rt], lambda e: e.activation(out=Rt.t[:], in_=Rt.t[:], func=AF.Silu))
                        RS = rms_rstd(RMSD, OFt, 256, 4)
                        for h in range(4):
                            S.op('dve', [OFt, RS, GN], [OFt], lambda e, h=h, RS=RS: e.scalar_tensor_tensor(
                                out=OFt.t[:, h * 256:(h + 1) * 256], in0=OFt.t[:, h * 256:(h + 1) * 256], scalar=RS.t[:, h:h + 1],
                                in1=GN.t[:], op0=ALU.mult, op1=ALU.mult))
                        S.op('dve', [OFt, Rt], [GBt], lambda e: e.tensor_tensor(out=GBt.t[:], in0=OFt.t[:], in1=Rt.t[:], op=ALU.mult))
                        pb = PB[tt % 2]
                        for j in range(8):
                            S.op('pe', [GBt, IDB], [pb], lambda e, pb=pb, j=j: e.transpose(
                                pb.t[:, j * 128:(j + 1) * 128], GBt.t[:, j * 128:(j + 1) * 128], IDB.t[:]))
                        S.op('act', [pb], [GT], lambda e, pb=pb, tt=tt: e.activation(
                            out=GT.t[:, :, tt * 128:(tt + 1) * 128], in_=pb.t[:].rearrange("p (k t) -> p k t", t=128), func=AF.Copy))
                    for g in range(4):
                        w = (2, 4, 8, 16)[g]
                        S.dma('sp', ICN, B_["invcnt"], lambda e, g=g: e.dma_start(
                            out=ICN.t[:], in_=invcnt[g:g + 1, t0:t0 + ST].to_broadcast([128, ST])))
                        for kc in range(2):
                            ch0 = (g * 2 + kc) * 128
                            S.op('dve', [], [UP], lambda e: e.memset(UP.t[:], 0.0))
                            lo = max(t0 - 8, 0)
                            hi = min(t0 + ST + 8, T)
                            S.dma('sp', UP, B_["UT"], lambda e, ch0=ch0, lo=lo, hi=hi: e.dma_start(
                                out=UP.t[:, lo - (t0 - 8):hi - (t0 - 8)], in_=UT[ch0:ch0 + 128, lo:hi]))
                            W_ = ST + 16
                            S.op('dve', [UP], [WA], lambda e: e.tensor_tensor(out=WA.t[:, 1:W_], in0=UP.t[:, 0:W_ - 1], in1=UP.t[:, 1:W_], op=ALU.add))
                            cur, oth = WA, WBb
                            lo_v, hi_v = 1, W_
                            sh = 1
                            while sh * 2 < w:
                                nlo, nhi = lo_v + sh, hi_v - sh
                                S.op('dve', [cur], [oth], lambda e, cur=cur, oth=oth, sh=sh, nlo=nlo, nhi=nhi: e.tensor_tensor(
                                    out=oth.t[:, nlo:nhi], in0=cur.t[:, nlo - sh:nhi - sh], in1=cur.t[:, nlo + sh:nhi + sh], op=ALU.add))
                                cur, oth = oth, cur
                                lo_v, hi_v = nlo, nhi
                                sh *= 2
                            assert lo_v <= 8 and hi_v >= ST + 8
                            S.op('dve', [cur, ICN], [oth], lambda e, cur=cur, oth=oth: e.tensor_tensor(
                                out=oth.t[:, 8:8 + ST], in0=cur.t[:, 8:8 + ST], in1=ICN.t[:], op=ALU.mult))
                            S.op('dve', [oth, UP], [PTb], lambda e, oth=oth, kc=kc: e.tensor_tensor(
                                out=PTb.t[:, kc, :], in0=oth.t[:, 8:8 + ST], in1=UP.t[:, 8:8 + ST], op=ALU.subtract))
                        for mo in range(2):
                            for nt in range(ST // 512):
                                pp = next_pf()
                                for kc in range(2):
                                    S.op('pe', [PW, PTb], [pp], lambda e, pp=pp, kc=kc, g=g, mo=mo, nt=nt: e.matmul(
                                        pp.t[:, :], lhsT=PW.t[:, g * 2 + kc, mo * 128:(mo + 1) * 128],
                                        rhs=PTb.t[:, kc, nt * 512:(nt + 1) * 512], start=(kc == 0), stop=(kc == 1)))
                                cc = g * 2 + mo
                                S.op('dve', [pp, PBC, PSC], [YPT], lambda e, pp=pp, cc=cc, nt=nt: e.tensor_scalar(
                                    out=YPT.t[:, cc, nt * 512:(nt + 1) * 512], in0=pp.t[:, :], scalar1=PBC.t[:, cc:cc + 1],
                                    scalar2=PSC.t[:, cc:cc + 1], op0=ALU.add, op1=ALU.mult))
                    for cg in range(4):
                        wa = WBF[cg % 2]
                        wbb = wa
                        S.dma('pool', wa, B_["bra_w"], lambda e, wa=wa, cg=cg: e.dma_start(
                            out=wa.t[:, 0:8, :], in_=bra_w[l, :, cg * 512:(cg + 1) * 512].rearrange("(k p) f -> p k f", p=128)))
                        S.dma('pool', wbb, B_["brb_w"], lambda e, wbb=wbb, cg=cg: e.dma_start(
                            out=wbb.t[:, 8:16, :], in_=brb_w[l, :, cg * 512:(cg + 1) * 512].rearrange("(k p) f -> p k f", p=128)))
                        for mt in range(4):
                            oc = cg * 4 + mt
                            for nt in range(ST // 512):
                                gat = GAt[nt % 2]
                                gbt = GBt2[nt % 2]
                                S.dma('sp', gat, B_["GAT"], lambda e, gat=gat, oc=oc, nt=nt: e.dma_start(
                                    out=gat.t[:], in_=GAT[oc * 128:(oc + 1) * 128, t0 + nt * 512:t0 + (nt + 1) * 512]))
                                S.dma('sp', gbt, B_["GBT"], lambda e, gbt=gbt, oc=oc, nt=nt: e.dma_start(
                                    out=gbt.t[:], in_=GBT[oc * 128:(oc + 1) * 128, t0 + nt * 512:t0 + (nt + 1) * 512]))
                                S.op('act', [gat], [gat], lambda e, gat=gat: e.activation(out=gat.t[:], in_=gat.t[:], func=AF.Sigmoid))
                                S.op('act', [gbt], [gbt], lambda e, gbt=gbt: e.activation(out=gbt.t[:], in_=gbt.t[:], func=AF.Sigmoid))
                                pa = next_pf()
                                for kc in range(8):
                                    S.op('pe', [wa, GT], [pa], lambda e, pa=pa, kc=kc, mt=mt, nt=nt, wa=wa: e.matmul(
                                        pa.t[:, :], lhsT=wa.t[:, kc, mt * 128:(mt + 1) * 128], rhs=GT.t[:, kc, nt * 512:(nt + 1) * 512],
                                        start=(kc == 0), stop=(kc == 7)))
                                pb2 = next_pf()
                                for kc in range(8):
                                    S.op('pe', [wbb, YPT], [pb2], lambda e, pb2=pb2, kc=kc, mt=mt, nt=nt, wbb=wbb: e.matmul(
                                        pb2.t[:, :], lhsT=wbb.t[:, 8 + kc, mt * 128:(mt + 1) * 128], rhs=YPT.t[:, kc, nt * 512:(nt + 1) * 512],
                                        start=(kc == 0), stop=(kc == 7)))
                                S.op('dve', [pa, gat], [YAs], lambda e, pa=pa, gat=gat: e.tensor_tensor(out=YAs.t[:], in0=pa.t[:], in1=gat.t[:], op=ALU.mult))
                                S.op('dve', [pb2, gbt], [gbt], lambda e, pb2=pb2, gbt=gbt: e.tensor_tensor(out=gbt.t[:], in0=pb2.t[:], in1=gbt.t[:], op=ALU.mult))
                                S.op('dve', [YAs, gbt], [MT], lambda e, gbt=gbt, oc=oc, nt=nt: e.tensor_tensor(
                                    out=MT.t[:, oc, nt * 512:(nt + 1) * 512], in0=YAs.t[:], in1=gbt.t[:], op=ALU.add))
                    def f2_load(n):
                        ng_, tt_ = divmod(n, TPS)
                        xtf_ = XTF[n % 2]
                        rr = t0 + tt_ * 128
                        S.dma('sp', xtf_, Xcur, lambda e, xtf_=xtf_, rr=rr, ng_=ng_: e.dma_start(
                            out=xtf_.t[:], in_=xcur_ap[rr:rr + 128, ng_ * 512:(ng_ + 1) * 512]))
                    f2_load(0)
                    for ng in range(4):
                        wo = WBF[ng % 2]
                        S.dma('pool', wo, B_["out_w"], lambda e, wo=wo, ng=ng: e.dma_start(
                            out=wo.t[:], in_=out_w[l, :, ng * 512:(ng + 1) * 512].rearrange("(k p) f -> p k f", p=128)))
                        for tt in range(TPS):
                            r0 = t0 + tt * 128
                            n_ = ng * TPS + tt
                            if n_ + 1 < 4 * TPS:
                                f2_load(n_ + 1)
                            xtf = XTF[n_ % 2]
                            pp = next_pf()
                            for kc in range(NKC):
                                S.op('pe', [MT, wo], [pp], lambda e, pp=pp, kc=kc, tt=tt, wo=wo: e.matmul(
                                    pp.t[:, :], lhsT=MT.t[:, kc, tt * 128:(tt + 1) * 128], rhs=wo.t[:, kc, :],
                                    start=(kc == 0), stop=(kc == NKC - 1)))
                            S.op('dve', [pp, G1BC], [YAs], lambda e, pp=pp, ng=ng: e.tensor_tensor(
                                out=YAs.t[:], in0=pp.t[:], in1=G1BC.t[:, ng * 512:(ng + 1) * 512], op=ALU.mult))
                            S.op('dve', [YAs, xtf], [xtf], lambda e, xtf=xtf: e.tensor_tensor(out=xtf.t[:], in0=YAs.t[:], in1=xtf.t[:], op=ALU.add))
                            S.dma('sp', Xnxt, xtf, lambda e, xtf=xtf, r0=r0, ng=ng: e.dma_start(
                                out=xnxt_ap[r0:r0 + 128, ng * 512:(ng + 1) * 512], in_=xtf.t[:]))
            S.barrier()
            Xcur, Xnxt = Xnxt, (B_["X0"] if Xnxt is B_["X1"] else B_["X1"])
            xcur_ap, xnxt_ap = xnxt_ap, (X0 if xnxt_ap is X1 else X1)

            with ExitStack() as pes:
                A2 = S.sb("a2bc", [128, D], F32, pes)
                B2 = S.sb("b2bc", [128, D], F32, pes)
                S.dma('sp', A2, B_["MOD"], lambda e: e.dma_start(out=A2.t[:], in_=MOD[4:5, :].to_broadcast([128, D])))
                S.dma('sp', B2, B_["ffn_g"], lambda e: e.dma_start(out=B2.t[:], in_=ffn_g[l:l + 1, :].to_broadcast([128, D])))
                S.op('dve', [A2, B2], [A2], lambda e: e.scalar_tensor_tensor(out=A2.t[:], in0=A2.t[:], scalar=1.0, in1=B2.t[:],
                                                                           op0=ALU.add, op1=ALU.mult))
                S.dma('sp', B2, B_["MOD"], lambda e: e.dma_start(out=B2.t[:], in_=MOD[3:4, :].to_broadcast([128, D])))
                WR = S.sb("wr", [128, NKC, 72], F32, pes)
                S.dma('sp', WR, B_["rc_w"], lambda e: e.dma_start(out=WR.t[:, :, 0:8], in_=rc_w[l].rearrange("(k p) f -> p k f", p=128)))
                S.dma('sp', WR, B_["rf_w"], lambda e: e.dma_start(out=WR.t[:, :, 8:72], in_=rf_w[l].rearrange("(k p) f -> p k f", p=128)))
                RB = S.sb("rbias", [128, 72], F32, pes)
                S.dma('sp', RB, B_["rc_b"], lambda e: e.dma_start(out=RB.t[:, 0:8], in_=rc_b[l:l + 1, :].to_broadcast([128, 8])))
                S.dma('sp', RB, B_["rf_b"], lambda e: e.dma_start(out=RB.t[:, 8:72], in_=rf_b[l:l + 1, :].to_broadcast([128, 64])))
                OH1 = S.sb("oh1", [128, NT, NEXP], BF16, pes)
                OH2 = S.sb("oh2", [128, NT, NEXP], BF16, pes)
                LOC = S.sb("loc", [128, NT, 2], F32, pes)
                WGT = S.sb("wgt", [128, NT, 2], F32, pes)
                DST = S.sb("dst", [128, NT, 2], I32, pes)
                BASE = S.sb("base", [128, NEXP], F32, pes)
                S.op('dve', [], [BASE], lambda e: e.memset(BASE.t[:], 0.0))
                XG = [S.sb(f"xg{i}", [128, D], F32, pes) for i in range(2)]
                H2 = S.sb("h2", [128, D], F32, pes)
                RMSG = (S.sb("n2_ss", [128, 1], F32, pes), H2)
                H2b = S.sb("h2b", [128, D], BF16, pes)
                H2T = S.sb("h2T", [128, NKC, 128], F32, pes)
                LG = S.sb("lg", [128, 72], F32, pes)
                OHE = S.sb("ohe", [128, NEXP], F32, pes)
                MM_ = S.sb("mm", [128, NEXP], F32, pes)
                M2 = S.sb("m2", [128, NEXP], F32, pes)
                SM = S.sb("sm", [128, 8], F32, pes)
                JK = S.sb("jk", [128, NEXP], F32, pes)
                AA = S.sb("aa", [128, NEXP], F32, pes)
                def g1_load(tt):
                    xt_ = XG[tt % 2]
                    S.dma('sp', xt_, Xcur, lambda e, xt_=xt_, tt=tt: e.dma_start(out=xt_.t[:], in_=xcur_ap[tt * 128:(tt + 1) * 128, :]))
                g1_load(0)
                for tt in range(NT):
                    r0 = tt * 128
                    xt = XG[tt % 2]
                    if tt + 1 < NT:
                        g1_load(tt + 1)
                    RS = rms_rstd(RMSG, xt, D, 1)
                    S.op('dve', [xt, RS, A2], [H2], lambda e, xt=xt, RS=RS: e.scalar_tensor_tensor(
                        out=H2.t[:], in0=xt.t[:], scalar=RS.t[:, 0:1], in1=A2.t[:], op0=ALU.mult, op1=ALU.mult))
                    S.op('dve', [H2, B2], [H2], lambda e: e.tensor_tensor(out=H2.t[:], in0=H2.t[:], in1=B2.t[:], op=ALU.add))
                    S.op('act', [H2], [H2b], lambda e: e.activation(out=H2b.t[:], in_=H2.t[:], func=AF.Copy))
                    S.dma('sp', B_["H2B"], H2b, lambda e, r0=r0: e.dma_start(out=H2B[r0:r0 + 128, :], in_=H2b.t[:]))
                    for q4 in range(4):
                        pt = next_pf()
                        for j in range(4):
                            kc = q4 * 4 + j
                            S.op('pe', [H2, CST], [pt], lambda e, pt=pt, j=j, kc=kc: e.transpose(
                                pt.t[:, j * 128:(j + 1) * 128], H2.t[:, kc * 128:(kc + 1) * 128], ident_f))
                        S.op('act', [pt], [H2T], lambda e, pt=pt, q4=q4: e.activation(
                            out=H2T.t[:, q4 * 4:(q4 + 1) * 4, :], in_=pt.t[:].rearrange("p (k t) -> p k t", t=128), func=AF.Copy))
                    pl = next_pf()
                    for kc in range(NKC):
                        S.op('pe', [H2T, WR], [pl], lambda e, pl=pl, kc=kc: e.matmul(
                            pl.t[:, 0:72], lhsT=H2T.t[:, kc, :], rhs=WR.t[:, kc, :], start=(kc == 0), stop=(kc == NKC - 1)))
                    S.op('dve', [pl, RB], [LG], lambda e, pl=pl: e.tensor_tensor(out=LG.t[:], in0=pl.t[:, 0:72], in1=RB.t[:], op=ALU.add))
                    S.op('dve', [LG], [SM], lambda e: e.tensor_reduce(out=SM.t[:, 0:1], in_=LG.t[:, 0:8], axis=mybir.AxisListType.X, op=ALU.max))
                    S.op('dve', [SM], [SM], lambda e: e.tensor_scalar(out=SM.t[:, 1:2], in0=SM.t[:, 0:1], scalar1=-1.0, scalar2=None, op0=ALU.mult))
                    S.op('act', [LG, SM], [JK, SM], lambda e: e.activation(out=JK.t[:, 0:8], in_=LG.t[:, 0:8], func=AF.Exp,
                                                                          bias=SM.t[:, 1:2], accum_out=SM.t[:, 2:3]))
                    S.op('dve', [SM], [SM], lambda e: e.reciprocal(out=SM.t[:, 3:4], in_=SM.t[:, 2:3]))
                    S.op('dve', [LG, SM], [JK], lambda e: e.tensor_scalar(out=JK.t[:, 8:16], in0=LG.t[:, 0:8], scalar1=SM.t[:, 0:1], scalar2=None, op0=ALU.is_ge))
                    for g in range(8):
                        S.op('dve', [JK, CST], [OHE], lambda e, g=g: e.tensor_scalar(
                            out=OHE.t[:, g * 8:(g + 1) * 8], in0=ones_f[:, 0:8], scalar1=JK.t[:, 8 + g:9 + g], scalar2=None, op0=ALU.mult))
                    S.op('dve', [LG, OHE], [MM_], lambda e: e.tensor_tensor(out=MM_.t[:], in0=LG.t[:, 8:72], in1=OHE.t[:], op=ALU.mult))
                    S.op('dve', [OHE], [JK], lambda e: e.tensor_scalar(out=JK.t[:], in0=OHE.t[:], scalar1=-1.0, scalar2=BIGNEG, op0=ALU.add, op1=ALU.mult))
                    S.op('dve', [MM_, JK], [MM_], lambda e: e.tensor_tensor(out=MM_.t[:], in0=MM_.t[:], in1=JK.t[:], op=ALU.add))
                    S.op('dve', [MM_], [SM], lambda e: e.tensor_reduce(out=SM.t[:, 4:5], in_=MM_.t[:], axis=mybir.AxisListType.X, op=ALU.max))
                    S.op('dve', [MM_, SM], [OH1], lambda e, tt=tt: e.tensor_scalar(out=OH1.t[:, tt, :], in0=MM_.t[:], scalar1=SM.t[:, 4:5], scalar2=None, op0=ALU.is_ge))
                    S.op('dve', [OH1, MM_], [M2], lambda e, tt=tt: e.scalar_tensor_tensor(out=M2.t[:], in0=OH1.t[:, tt, :], scalar=-BIGNEG, in1=MM_.t[:],
                                                                                          op0=ALU.mult, op1=ALU.add))
                    S.op('dve', [M2], [SM], lambda e: e.tensor_reduce(out=SM.t[:, 5:6], in_=M2.t[:], axis=mybir.AxisListType.X, op=ALU.max))
                    S.op('dve', [M2, SM], [OH2], lambda e, tt=tt: e.tensor_scalar(out=OH2.t[:, tt, :], in0=M2.t[:], scalar1=SM.t[:, 5:6], scalar2=None, op0=ALU.is_ge))
                    S.op('dve', [SM], [SM], lambda e: e.tensor_tensor(out=SM.t[:, 6:7], in0=SM.t[:, 5:6], in1=SM.t[:, 4:5], op=ALU.subtract))
                    S.op('act', [SM], [SM], lambda e: e.activation(out=SM.t[:, 6:7], in_=SM.t[:, 6:7], func=AF.Exp))
                    S.op('dve', [SM], [SM], lambda e: e.tensor_scalar(out=SM.t[:, 7:8], in0=SM.t[:, 6:7], scalar1=1.0, scalar2=None, op0=ALU.add))
                    S.op('dve', [SM], [SM], lambda e: e.reciprocal(out=SM.t[:, 7:8], in_=SM.t[:, 7:8]))
                    S.op('dve', [SM], [WGT], lambda e, tt=tt: e.tensor_tensor(out=WGT.t[:, tt, 0:1], in0=SM.t[:, 7:8], in1=SM.t[:, 3:4], op=ALU.mult))
                    S.op('dve', [SM, WGT], [WGT], lambda e, tt=tt: e.tensor_tensor(out=WGT.t[:, tt, 1:2], in0=WGT.t[:, tt, 0:1], in1=SM.t[:, 6:7], op=ALU.mult))
                    S.op('dve', [OH1, OH2], [AA], lambda e, tt=tt: e.tensor_tensor(out=AA.t[:], in0=OH1.t[:, tt, :], in1=OH2.t[:, tt, :], op=ALU.add))
                    pq = next_pf()
                    S.op('pe', [AA, CST], [pq], lambda e, pq=pq: e.matmul(pq.t[:, 0:64], lhsT=TRI[('b', 'B')], rhs=AA.t[:], start=True, stop=True))
                    S.op('pe', [AA, CST], [pq], lambda e, pq=pq: e.matmul(pq.t[:, 64:128], lhsT=ones_f, rhs=AA.t[:], start=True, stop=True))
                    S.op('dve', [pq, BASE], [JK], lambda e, pq=pq: e.scalar_tensor_tensor(out=JK.t[:], in0=pq.t[:, 0:64], scalar=-16.0, in1=BASE.t[:],
                                                                                         op0=ALU.mult, op1=ALU.add))
                    S.op('dve', [JK, OH1], [M2, LOC], lambda e, tt=tt: e.scalar_tensor_tensor(
                        out=M2.t[:], in0=JK.t[:], scalar=1.0, in1=OH1.t[:, tt, :], op0=ALU.mult, op1=ALU.mult, accum_out=LOC.t[:, tt, 0:1]))
                    S.op('dve', [JK, OH2], [M2, LOC], lambda e, tt=tt: e.scalar_tensor_tensor(
                        out=M2.t[:], in0=JK.t[:], scalar=1.0, in1=OH2.t[:, tt, :], op0=ALU.mult, op1=ALU.mult, accum_out=LOC.t[:, tt, 1:2]))
                    S.op('dve', [pq, BASE], [BASE], lambda e, pq=pq: e.tensor_tensor(out=BASE.t[:], in0=BASE.t[:], in1=pq.t[:, 64:128], op=ALU.add))
                NBK = S.sb("nbk", [128, NEXP], F32, pes)
                CS0 = S.sb("cs0", [128, NEXP], F32, pes)
                CS1 = S.sb("cs1", [128, NEXP], F32, pes)
                PST = S.sb("pst", [128, NEXP], F32, pes)
                BEX = S.sb("bex", [128, NBLK], F32, pes)
                BEXI = S.sb("bexi", [128, NBLK], I32, pes)
                S.op('dve', [], [NBK], lambda e: e.memset(NBK.t[:], 0.0))
                for k in range(T // BLK):
                    S.op('dve', [BASE, NBK], [NBK], lambda e, k=k: e.scalar_tensor_tensor(
                        out=NBK.t[:], in0=BASE.t[:], scalar=float(k * BLK), in1=NBK.t[:], op0=ALU.is_gt, op1=ALU.add))
                S.op('dve', [NBK], [CS0], lambda e: e.tensor_copy(out=CS0.t[:], in_=NBK.t[:]))
                cur, oth = CS0, CS1
                sh = 1
                while sh < NEXP:
                    S.op('dve', [cur], [oth], lambda e, cur=cur, oth=oth: e.tensor_copy(out=oth.t[:], in_=cur.t[:]))
                    S.op('dve', [cur], [oth], lambda e, cur=cur, oth=oth, sh=sh: e.tensor_tensor(
                        out=oth.t[:, sh:NEXP], in0=cur.t[:, sh:NEXP], in1=cur.t[:, 0:NEXP - sh], op=ALU.add))
                    cur, oth = oth, cur
                    sh *= 2
                PEND = cur
                S.op('dve', [PEND, NBK], [PST], lambda e, PEND=PEND: e.tensor_tensor(out=PST.t[:], in0=PEND.t[:], in1=NBK.t[:], op=ALU.subtract))
                for b in range(NBLK):
                    S.op('dve', [PEND], [JK, BEX], lambda e, b=b, PEND=PEND: e.tensor_scalar(
                        out=JK.t[:], in0=PEND.t[:], scalar1=float(b), scalar2=0.0, op0=ALU.is_le, op1=ALU.add, accum_out=BEX.t[:, b:b + 1]))
                SKF = S.sb("skf", [128, NBLK], F32, pes)
                S.op('dve', [BEX], [SKF], lambda e: e.tensor_scalar(out=SKF.t[:], in0=BEX.t[:], scalar1=float(NEXP) - 0.5, scalar2=(1.0e7 if SKIP_OOB else 0.0),
                                                                     op0=ALU.is_ge, op1=ALU.mult))
                S.op('dve', [BEX], [BEX], lambda e: e.tensor_scalar(out=BEX.t[:], in0=BEX.t[:], scalar1=float(NEXP - 1), scalar2=None, op0=ALU.min))
                S.op('dve', [BEX], [BEXI], lambda e: e.tensor_copy(out=BEXI.t[:], in_=BEX.t[:]))
                for tt in range(NT):
                    for r in range(2):
                        OH = OH1 if r == 0 else OH2
                        S.op('dve', [PST, OH], [M2, SM], lambda e, tt=tt, OH=OH: e.scalar_tensor_tensor(
                            out=M2.t[:], in0=PST.t[:], scalar=float(BLK), in1=OH.t[:, tt, :], op0=ALU.mult, op1=ALU.mult, accum_out=SM.t[:, 0:1]))
                        S.op('dve', [SM, LOC], [SM], lambda e, tt=tt, r=r: e.tensor_tensor(out=SM.t[:, 1:2], in0=SM.t[:, 0:1], in1=LOC.t[:, tt, r:r + 1], op=ALU.add))
                        S.op('dve', [SM], [DST], lambda e, tt=tt, r=r: e.tensor_copy(out=DST.t[:, tt, r:r + 1], in_=SM.t[:, 1:2]))
                XB = [S.sb(f"xb{i}", [128, D], BF16, pes) for i in range(2)]
                for tt in range(NT):
                    r0 = tt * 128
                    xb = XB[tt % 2]
                    S.dma('sp', xb, B_["H2B"], lambda e, xb=xb, r0=r0: e.dma_start(out=xb.t[:], in_=H2B[r0:r0 + 128, :]))
                    for r in range(2):
                        S.dma('pool', B_["XS"], [xb, DST], lambda e, xb=xb, tt=tt, r=r: e.indirect_dma_start(
                            out=XS[:, :], out_offset=bass.IndirectOffsetOnAxis(ap=DST.t[:, tt, r:r + 1], axis=0),
                            in_=xb.t[:], in_offset=None))
                W13 = [S.sb(f"w13{i}", [128, NKC, 512], BF16, pes) for i in range(3)]
                W2 = [S.sb(f"w2{i}", [128, 2, D], BF16, pes) for i in range(3)]
                XBT = S.sb("xbT", [128, NKC, 128], BF16, pes)
                ACTf = S.sb("actf", [128, FF], F32, pes)
                ACTb = S.sb("actb", [128, FF], BF16, pes)
                ACTT = S.sb("actT", [128, 2, 128], BF16, pes)
                YSB = [S.sb(f"ysb{i}", [128, D], F32, pes) for i in range(2)]
                E5 = S.sb("e512", [128, NBLK], F32, pes)
                E2G = S.sb("e256", [128, NBLK], F32, pes)
                IDXA = S.sb("idxa", [128, NBLK, 4], I32, pes)
                IDXB = S.sb("idxb", [128, NBLK, 2], I32, pes)
                KPL = S.sb("kpl", [128, 8], F32, pes)
                S.op('dve', [CST], [KPL], lambda e: e.tensor_scalar(out=KPL.t[:, 0:4], in0=CST.t[:, 768:772], scalar1=(0.25 if PRECAST else float(l * NEXP * 512)), scalar2=None, op0=(ALU.mult if PRECAST else ALU.add)))
                S.op('dve', [CST], [KPL], lambda e: e.tensor_scalar(out=KPL.t[:, 4:6], in0=CST.t[:, 772:774], scalar1=(0.0 if PRECAST else float(l * NEXP * 256)), scalar2=None, op0=ALU.add))
                S.op('dve', [BEX, SKF], [E5], lambda e: e.scalar_tensor_tensor(out=E5.t[:], in0=BEX.t[:], scalar=(128.0 if PRECAST else 512.0), in1=SKF.t[:], op0=ALU.mult, op1=ALU.add))
                S.op('dve', [BEX, SKF], [E2G], lambda e: e.scalar_tensor_tensor(out=E2G.t[:], in0=BEX.t[:], scalar=256.0, in1=SKF.t[:], op0=ALU.mult, op1=ALU.add))
                for b in range(NBLK):
                    S.op('dve', [E5, KPL], [IDXA], lambda e, b=b: e.scalar_tensor_tensor(
                        out=IDXA.t[:, b, :], in0=ones_f[:, 0:4], scalar=E5.t[:, b:b + 1], in1=KPL.t[:, 0:4], op0=ALU.mult, op1=ALU.add))
                    S.op('dve', [E2G, KPL], [IDXB], lambda e, b=b: e.scalar_tensor_tensor(
                        out=IDXB.t[:, b, :], in0=ones_f[:, 0:2], scalar=E2G.t[:, b:b + 1], in1=KPL.t[:, 4:6], op0=ALU.mult, op1=ALU.add))
                XBT2 = [XBT, S.sb("xbT1", [128, NKC, 128], BF16, pes)]
                ACTf2 = [ACTf, S.sb("actf1", [128, FF], F32, pes)]
                ACTb2 = [ACTb, S.sb("actb1", [128, FF], BF16, pes)]
                ACTT2 = [ACTT, S.sb("actT1", [128, 2, 128], BF16, pes)]
                NSB = NBLK * NSUB

                def g3_load(n):
                    xb_ = XB[n % 2]
                    S.dma('sp', xb_, B_["XS"], lambda e, xb_=xb_, n=n: e.dma_start(out=xb_.t[:], in_=XS[n * 128:(n + 1) * 128, :]))

                def g3_weights(b):
                    w13_, w2_ = W13[b % 3], W2[b % 3]
                    S.dma('pool', w13_, [B_["EW13B"], IDXA], lambda e, w13_=w13_, b=b: e.indirect_dma_start(
                        out=w13_.t[:].rearrange("p k f -> p (k f)"), out_offset=None, in_=EW13B[:, :],
                        in_offset=bass.IndirectOffsetOnAxis(ap=IDXA.t[:, b, 0:1], axis=0)))
                    S.dma('pool', w2_, [B_["EW2B"], IDXA], lambda e, w2_=w2_, b=b: e.indirect_dma_start(
                        out=w2_.t[:].rearrange("p k f -> p (k f)"), out_offset=None, in_=EW2B[:, :],
                        in_offset=bass.IndirectOffsetOnAxis(ap=IDXA.t[:, b, 0:1], axis=0)))

                PHS = {}

                def stA(n):
                    xb = XB[n % 2]
                    xbt = XBT2[n % 2]
                    for half in range(2):
                        pb = PB[half]
                        for j in range(8):
                            kc = half * 8 + j
                            S.op('pe', [xb, IDB], [pb], lambda e, pb=pb, j=j, kc=kc, xb=xb: e.transpose(
                                pb.t[:, j * 128:(j + 1) * 128], xb.t[:, kc * 128:(kc + 1) * 128], IDB.t[:]))
                        if half == 0:
                            S.op('act', [pb], [xbt], lambda e, pb=pb, half=half, xbt=xbt: e.activation(
                                out=xbt.t[:, half * 8:(half + 1) * 8, :], in_=pb.t[:].rearrange("p (k t) -> p k t", t=128), func=AF.Copy))
                        else:
                            S.op('dve', [pb], [xbt], lambda e, pb=pb, half=half, xbt=xbt: e.tensor_copy(
                                out=xbt.t[:, half * 8:(half + 1) * 8, :], in_=pb.t[:].rearrange("p (k t) -> p k t", t=128)))

                def stB(n):
                    w13 = W13[(n // NSUB) % 3]
                    xbt, actf, actb = XBT2[n % 2], ACTf2[n % 2], ACTb2[n % 2]
                    ph = next_pf()
                    for kc in range(NKC):
                        S.op('pe', [xbt, w13], [ph], lambda e, ph=ph, kc=kc, w13=w13, xbt=xbt: e.matmul(
                            ph.t[:, :], lhsT=xbt.t[:, kc, :], rhs=w13.t[:, kc, :], start=(kc == 0), stop=(kc == NKC - 1)))
                    S.op('act', [ph], [actf], lambda e, ph=ph, actf=actf: e.activation(out=actf.t[:], in_=ph.t[:, 0:FF], func=AF.Silu))
                    S.op('dve', [actf, ph], [actb], lambda e, ph=ph, actf=actf, actb=actb: e.tensor_tensor(
                        out=actb.t[:], in0=actf.t[:], in1=ph.t[:, FF:2 * FF], op=ALU.mult))

                def stC(n):
                    actb, actt = ACTb2[n % 2], ACTT2[n % 2]
                    pbt = PB[2]
                    for j in range(2):
                        S.op('pe', [actb, IDB], [pbt], lambda e, j=j, pbt=pbt, actb=actb: e.transpose(
                            pbt.t[:, j * 128:(j + 1) * 128], actb.t[:, j * 128:(j + 1) * 128], IDB.t[:]))
                    S.op('act', [pbt], [actt], lambda e, pbt=pbt, actt=actt: e.activation(
                        out=actt.t[:], in_=pbt.t[:, 0:256].rearrange("p (k t) -> p k t", t=128), func=AF.Copy))

                def stD(n):
                    w2 = W2[(n // NSUB) % 3]
                    actt = ACTT2[n % 2]
                    ysb = YSB[n % 2]
                    for nt in range(4):
                        py = next_pf()
                        for kc in range(2):
                            S.op('pe', [actt, w2], [py], lambda e, py=py, kc=kc, nt=nt, w2=w2, actt=actt: e.matmul(
                                py.t[:, :], lhsT=actt.t[:, kc, :], rhs=w2.t[:, kc, nt * 512:(nt + 1) * 512], start=(kc == 0), stop=(kc == 1)))
                        if nt % 2 == 0:
                            S.op('act', [py], [ysb], lambda e, py=py, nt=nt, ysb=ysb: e.activation(out=ysb.t[:, nt * 512:(nt + 1) * 512], in_=py.t[:], func=AF.Copy))
                        else:
                            S.op('dve', [py], [ysb], lambda e, py=py, nt=nt, ysb=ysb: e.tensor_copy(out=ysb.t[:, nt * 512:(nt + 1) * 512], in_=py.t[:]))
                    for hh in range(2):
                        S.dma('sp', B_["YS"], ysb, lambda e, ysb=ysb, n=n, hh=hh: e.dma_start(
                            out=YSH[hh][n * 128:(n + 1) * 128, :], in_=ysb.t[:, hh * 1024:(hh + 1) * 1024]))

                g3_load(0)
                for b0 in range(min(3, NBLK)):
                    g3_weights(b0)
                for it in range(NSB + 3):
                    nA, nB, nC, nD = it, it - 1, it - 2, it - 3
                    if nD >= 0 and nD % NSUB == 0:
                        bD = nD // NSUB
                        if bD >= 1 and bD + 2 < NBLK:
                            g3_weights(bD + 2)
                    if nA < NSB:
                        if nA + 1 < NSB:
                            g3_load(nA + 1)
                        stA(nA)
                    if 0 <= nB < NSB:
                        stB(nB)
                    if 0 <= nC < NSB:
                        stC(nC)
                    if 0 <= nD < NSB:
                        stD(nD)
                G2BC = A2
                S.dma('sp', G2BC, B_["MOD"], lambda e: e.dma_start(out=G2BC.t[:], in_=MOD[5:6, :].to_broadcast([128, D])))
                YA = [YSB[0], H2]
                YB = [YSB[1], Buf("h2t_alias", H2T.t[:].rearrange("p k t -> p (k t)"))]

                def g4_loads(tt):
                    xt_ = XG[tt % 2]
                    S.dma('sp', xt_, Xcur, lambda e, xt_=xt_, tt=tt: e.dma_start(out=xt_.t[:], in_=xcur_ap[tt * 128:(tt + 1) * 128, :]))
                    for r, yb_ in ((0, YA[tt % 2]), (1, YB[tt % 2])):
                        for hh in range(2):
                            S.dma('pool', yb_, [B_["YS"], DST], lambda e, yb_=yb_, tt=tt, r=r, hh=hh: e.indirect_dma_start(
                                out=yb_.t[:, hh * 1024:(hh + 1) * 1024], out_offset=None, in_=YSH[hh][:, :],
                                in_offset=bass.IndirectOffsetOnAxis(ap=DST.t[:, tt, r:r + 1], axis=0)))

                g4_loads(0)
                for tt in range(NT):
                    r0 = tt * 128
                    if tt + 1 < NT:
                        g4_loads(tt + 1)
                    xt = XG[tt % 2]
                    y0, y1 = YA[tt % 2], YB[tt % 2]
                    S.op('dve', [y0, WGT], [y0], lambda e, y0=y0, tt=tt: e.tensor_scalar(out=y0.t[:], in0=y0.t[:], scalar1=WGT.t[:, tt, 0:1], scalar2=None, op0=ALU.mult))
                    S.op('dve', [y1, WGT, y0], [y0], lambda e, y0=y0, y1=y1, tt=tt: e.scalar_tensor_tensor(
                        out=y0.t[:], in0=y1.t[:], scalar=WGT.t[:, tt, 1:2], in1=y0.t[:], op0=ALU.mult, op1=ALU.add))
                    S.op('dve', [y0, G2BC], [y0], lambda e, y0=y0: e.tensor_tensor(out=y0.t[:], in0=y0.t[:], in1=G2BC.t[:], op=ALU.mult))
                    S.op('dve', [y0, xt], [xt], lambda e, y0=y0, xt=xt: e.tensor_tensor(out=xt.t[:], in0=y0.t[:], in1=xt.t[:], op=ALU.add))
                    S.dma('sp', Xnxt, xt, lambda e, xt=xt, r0=r0: e.dma_start(out=xnxt_ap[r0:r0 + 128, :], in_=xt.t[:]))
            S.barrier()
            Xcur, Xnxt = Xnxt, (B_["X0"] if Xnxt is B_["X1"] else B_["X1"])
            xcur_ap, xnxt_ap = xnxt_ap, (X0 if xnxt_ap is X1 else X1)

        with ExitStack() as pes:
            FG = S.sb("fgbc", [128, D], F32, pes)
            S.dma('sp', FG, B_["fin_g"], lambda e: e.dma_start(out=FG.t[:], in_=fin_g[0:1, :].to_broadcast([128, D])))
            XF = [S.sb(f"xf{i}", [128, D], F32, pes) for i in range(2)]
            RMSF = rms_alloc(pes, D, 1, "nf")
            def fin_load(tt):
                xt_ = XF[tt % 2]
                S.dma('sp', xt_, Xcur, lambda e, xt_=xt_, tt=tt: e.dma_start(out=xt_.t[:], in_=xcur_ap[tt * 128:(tt + 1) * 128, :]))
            fin_load(0)
            for tt in range(NT):
                r0 = tt * 128
                xt = XF[tt % 2]
                if tt + 1 < NT:
                    fin_load(tt + 1)
                RS = rms_rstd(RMSF, xt, D, 1)
                S.op('dve', [xt, RS, FG], [xt], lambda e, xt=xt, RS=RS: e.scalar_tensor_tensor(
                    out=xt.t[:], in0=xt.t[:], scalar=RS.t[:, 0:1], in1=FG.t[:], op0=ALU.mult, op1=ALU.mult))
                S.dma('sp', B_["y"], xt, lambda e, xt=xt, r0=r0: e.dma_start(out=y_out[r0:r0 + 128, :], in_=xt.t[:]))
        S.finish()
    return nc


def make_consts(T):
    i = np.arange(128)
    s = i[:, None]
    t = i[None, :]
    cst = np.zeros((128, 6 * 128 + 8), np.float32)
    for q in range(4):
        cst[:, 768 + q] = 4 * i + q
    for kc in range(2):
        cst[:, 772 + kc] = 2 * i + kc
    cst[:, 0:128] = np.eye(128, dtype=np.float32)
    f = -1.0 / 16.0
    cst[:, 128:256] = f * (s <= t)
    cst[:, 256:384] = f * (s > t)
    cst[:, 384:512] = f * (s >= t)
    cst[:, 512:640] = f * (s < t)
    cst[:, 640:768] = 1.0
    cm = np.zeros((128, 1024), np.float32)
    mf = (s <= t).astype(np.float32)
    mb = (s > t).astype(np.float32)
    for h in range(4):
        cm[:, h * 128:(h + 1) * 128] = mf
        cm[:, 512 + h * 128:512 + (h + 1) * 128] = mb
    tt = np.arange(T)
    ic = np.zeros((4, T), np.float32)
    for g, w in enumerate((2, 4, 8, 16)):
        lo = np.clip(tt - w // 2, 0, T)
        hi = np.clip(tt + w // 2, 0, T)
        ic[g] = 1.0 / (hi - lo).astype(np.float32)
    return cst, cm, ic


_PROG = {}


def run(inputs, T, L, ncores, dbg=()):
    key = (T, L, tuple(dbg))
    if key not in _PROG:
        _PROG[key] = build_program(T, L, dbg)
    nc = _PROG[key]
    cst, cm, ic = make_consts(T)
    f = lambda a: np.ascontiguousarray(np.asarray(a, dtype=np.float32))
    shared = {k: f(inputs[k]) for k in (
        "ada_w", "ada_b", "mix_norm_g", "in_w", "decay_fw_w", "decay_fw_b", "decay_bw_w", "decay_bw_b", "gla_norm_g",
        "pool_w", "pool_b", "pool_scale", "branch_a_w", "branch_b_w", "out_w", "ffn_norm_g", "router_coarse_w",
        "router_coarse_b", "router_fine_w", "router_fine_b")}
    shared = {k: v[:L] for k, v in shared.items()}
    w1 = f(inputs["expert_w1"])[:L]
    w3 = f(inputs["expert_w3"])[:L]
    w2 = f(inputs["expert_w2"])[:L]
    w13 = np.concatenate([w1, w3], axis=-1).reshape(L, NEXP, NKC, 128, 2 * FF).transpose(0, 1, 3, 2, 4)
    shared["ew13"] = np.ascontiguousarray(w13).reshape(L * NEXP * 128 * 4, 2048)
    del w13, w1, w3
    w2h = w2.reshape(L, NEXP, 2, 128, D).transpose(0, 1, 3, 2, 4)
    shared["ew2h"] = np.ascontiguousarray(w2h).reshape(L * NEXP * 128 * 2, 2048)
    del w2h, w2
    shared["final_norm_g"] = f(inputs["final_norm_g"]).reshape(1, D)
    shared["consts"] = cst
    shared["cmask"] = cm
    shared["invcnt"] = ic
    x = f(inputs["x"])
    c = f(inputs["c"])
    in_maps = []
    for b in range(ncores):
        m = dict(shared)
        m["x"] = x[b]
        m["c"] = c[b:b + 1]
        in_maps.append(m)
    res = run_bass_kernel_spmd(nc, in_maps, core_ids=list(range(ncores)), **RUN_KW)
    return res


def kernel(**inputs):
    x = np.asarray(inputs["x"])
    Bn, T, _ = x.shape
    L = np.asarray(inputs["ada_w"]).shape[0]
    res = run(inputs, T, L, Bn)
    return np.stack([np.asarray(res.results[b]["y"], dtype=np.float32) for b in range(Bn)], axis=0)
```
